# Optimizing a Trainium2 kernel written in Bass

```python
import math
import jax, jax.numpy as jnp
from jax import lax
import numpy as np


D_MODEL = 1024
BATCH = 8
SEQ = 2048
DEPTH = 2

N_A_LAYERS = DEPTH // 2
N_B_LAYERS = DEPTH - N_A_LAYERS

A_CHUNK = 128
A_GROUPS = 8
A_WIDTH = 2 * D_MODEL
A_GROUP_DIM = A_WIDTH // A_GROUPS

B_HEADS = 16
B_HEAD_DIM = D_MODEL // B_HEADS
B_BLOCK = 256
B_TOPK = 3
B_QCHUNK = 32

N_EXPERTS = 64
TOP_K = 8
N_GROUPS = 8
TOPK_GROUPS = 4
EXPERT_DIM = 256
SHARED_DIM = 256
ROUTED_SCALE = 2.5
MOE_ROW_BLOCK = 128

RMS_EPS = 1e-6
LN_EPS = 1e-5
NEG_INF = -1e30

kernel_name = 'hybrid_gmlp_moba_moe_yoco'


def rmsnorm(x, g):
    xf = x.astype(jnp.float32)
    y = xf * lax.rsqrt(jnp.mean(xf * xf, axis=-1, keepdims=True) + RMS_EPS)
    return y.astype(x.dtype) * g


def layernorm(x, g, b):
    xf = x.astype(jnp.float32)
    mu = jnp.mean(xf, axis=-1, keepdims=True)
    var = jnp.mean(jnp.square(xf - mu), axis=-1, keepdims=True)
    y = (xf - mu) * lax.rsqrt(var + LN_EPS)
    return y.astype(x.dtype) * g + b


def ada(c, w, b, n):
    m = jax.nn.silu(c) @ w + b
    return [t[:, None, :] for t in jnp.split(m, n, axis=-1)]


def modulate(h, shift, scale):
    return h * (1 + scale) + shift


def swiglu(x, wg, wu, wd):
    return (jax.nn.silu(x @ wg) * (x @ wu)) @ wd


def gmlp_mixer(h, w_in, b_in, ln_g, ln_b, w_s, b_s, w_out):
    B, S, _ = h.shape
    z = jax.nn.gelu(h @ w_in + b_in)
    u, v = jnp.split(z, 2, axis=-1)
    v = layernorm(v, ln_g, ln_b)
    nc = S // A_CHUNK
    v = v.reshape(B, nc, A_CHUNK, A_GROUPS, A_GROUP_DIM)
    causal = jnp.tril(jnp.ones((A_CHUNK, A_CHUNK), dtype=bool))
    w = jnp.where(causal[None], w_s, 0)
    sv = jnp.einsum('gts,bcsgd->bctgd', w, v) + b_s.T[None, None, :, :, None]
    y = u * sv.reshape(B, S, A_WIDTH)
    return y @ w_out


def shared_kv(x, c, g, w_ada, b_ada, w_k, w_v):
    shift, scale = ada(c, w_ada, b_ada, 2)
    h = modulate(rmsnorm(x, g), shift, scale)
    B, S, _ = h.shape
    k = (h @ w_k).reshape(B, S, B_HEADS, B_HEAD_DIM).transpose(0, 2, 1, 3)
    v = (h @ w_v).reshape(B, S, B_HEADS, B_HEAD_DIM).transpose(0, 2, 1, 3)
    nb = -(-S // B_BLOCK)
    pad = nb * B_BLOCK - S
    k = jnp.pad(k, ((0, 0), (0, 0), (0, pad), (0, 0)))
    v = jnp.pad(v, ((0, 0), (0, 0), (0, pad), (0, 0)))
    kb = k.reshape(B, B_HEADS, nb, B_BLOCK, B_HEAD_DIM)
    vb = v.reshape(B, B_HEADS, nb, B_BLOCK, B_HEAD_DIM)
    kmean = jnp.mean(kb, axis=3)
    return kb, vb, kmean


def moba_attention(q, kb, vb, kmean):
    B, H, S, dh = q.shape
    nb = kb.shape[2]
    n_sel = min(B_TOPK, nb - 1)
    scale = dh ** -0.5
    nqc = S // B_QCHUNK
    qc = q.reshape(B, H, nqc, B_QCHUNK, dh).transpose(0, 2, 1, 3, 4)
    head_idx = jnp.arange(H)[:, None, None]

    def one_seq(args):
        q_s, kb_s, vb_s, km_s = args

        def one_chunk(args2):
            ci, q_c = args2
            start = ci * B_QCHUNK
            own = start // B_BLOCK
            qpos = start + jnp.arange(B_QCHUNK)
            kpos = own * B_BLOCK + jnp.arange(B_BLOCK)
            k_own = lax.dynamic_index_in_dim(kb_s, own, axis=1, keepdims=False)
            v_own = lax.dynamic_index_in_dim(vb_s, own, axis=1, keepdims=False)
            s_own = jnp.einsum('hqd,hjd->hqj', q_c, k_own).astype(jnp.float32) * scale
            s_own = jnp.where((kpos[None, :] <= qpos[:, None])[None], s_own, NEG_INF)
            if n_sel > 0:
                gate = jnp.einsum('hqd,hnd->hqn', q_c, km_s).astype(jnp.float32)
                past = jnp.arange(nb) < own
                gate = jnp.where(past[None, None, :], gate, NEG_INF)
                _, idx = lax.top_k(gate, n_sel)
                valid = idx < own
                k_sel = kb_s[head_idx, idx]
                v_sel = vb_s[head_idx, idx]
                s_sel = jnp.einsum('hqd,hqnjd->hqnj', q_c, k_sel).astype(jnp.float32) * scale
                s_sel = jnp.where(valid[..., None], s_sel, NEG_INF)
                s = jnp.concatenate([s_sel.reshape(H, B_QCHUNK, n_sel * B_BLOCK), s_own], axis=-1)
                p = jax.nn.softmax(s, axis=-1).astype(v_own.dtype)
                p_sel = p[..., :n_sel * B_BLOCK].reshape(H, B_QCHUNK, n_sel, B_BLOCK)
                p_own = p[..., n_sel * B_BLOCK:]
                o = (jnp.einsum('hqnj,hqnjd->hqd', p_sel, v_sel)
                     + jnp.einsum('hqj,hjd->hqd', p_own, v_own))
            else:
                p = jax.nn.softmax(s_own, axis=-1).astype(v_own.dtype)
                o = jnp.einsum('hqj,hjd->hqd', p, v_own)
            return o

        return lax.map(one_chunk, (jnp.arange(nqc), q_s))

    o = lax.map(one_seq, (qc, kb, vb, kmean))
    return o.transpose(0, 2, 1, 3, 4).reshape(B, H, S, dh)


def moba_mixer(h, w_q, w_o, kb, vb, kmean):
    B, S, D = h.shape
    q = (h @ w_q).reshape(B, S, B_HEADS, B_HEAD_DIM).transpose(0, 2, 1, 3)
    o = moba_attention(q, kb, vb, kmean)
    return o.transpose(0, 2, 1, 3).reshape(B, S, D) @ w_o


def grouped_experts(xt, eidx, w, w_gate, w_up, w_down):
    T, D = xt.shape
    K = eidx.shape[1]
    E, M = N_EXPERTS, MOE_ROW_BLOCK
    TK = T * K
    e_flat = eidx.reshape(TK)
    tok_flat = jnp.arange(TK, dtype=jnp.int32) // K
    w_flat = w.reshape(TK)
    order = jnp.argsort(e_flat)
    e_sorted = e_flat[order]
    tok_sorted = tok_flat[order]
    w_sorted = w_flat[order]
    counts = jnp.zeros((E,), jnp.int32).at[e_flat].add(1)
    starts = jnp.cumsum(counts) - counts
    padded = ((counts + M - 1) // M) * M
    pad_ends = jnp.cumsum(padded)
    pad_starts = pad_ends - padded
    rank = jnp.arange(TK, dtype=jnp.int32) - starts[e_sorted]
    dest = pad_starts[e_sorted] + rank
    n_blocks = -(-(TK + E * (M - 1)) // M)
    R = n_blocks * M
    x_pad = jnp.zeros((R, D), xt.dtype).at[dest].set(xt[tok_sorted])
    block_start = jnp.arange(n_blocks, dtype=jnp.int32) * M
    block_expert = jnp.minimum(jnp.sum(pad_ends[None, :] <= block_start[:, None], axis=1), E - 1)

    def run_block(args):
        xb, e = args
        return swiglu(xb, w_gate[e], w_up[e], w_down[e])

    y_pad = lax.map(run_block, (x_pad.reshape(n_blocks, M, D), block_expert)).reshape(R, D)
    y = y_pad[dest] * w_sorted[:, None].astype(xt.dtype)
    return jax.ops.segment_sum(y, tok_sorted, num_segments=T)


def moe(h, w_router, e_bias, w_gate, w_up, w_down, ws_gate, ws_up, ws_down):
    B, S, D = h.shape
    T = B * S
    xt = h.reshape(T, D)
    scores = jax.nn.sigmoid((xt @ w_router).astype(jnp.float32))
    choice = scores + e_bias.astype(jnp.float32)
    grp = choice.reshape(T, N_GROUPS, N_EXPERTS // N_GROUPS)
    grp_score = jnp.sum(lax.top_k(grp, 2)[0], axis=-1)
    _, gidx = lax.top_k(grp_score, TOPK_GROUPS)
    gmask = jnp.sum(jax.nn.one_hot(gidx, N_GROUPS, dtype=jnp.float32), axis=1) > 0
    emask = jnp.repeat(gmask, N_EXPERTS // N_GROUPS, axis=1)
    choice = jnp.where(emask, choice, NEG_INF)
    _, eidx = lax.top_k(choice, TOP_K)
    w = jnp.take_along_axis(scores, eidx, axis=1)
    w = w / jnp.sum(w, axis=-1, keepdims=True) * ROUTED_SCALE
    routed = grouped_experts(xt, eidx, w, w_gate, w_up, w_down)
    shared = swiglu(xt, ws_gate, ws_up, ws_down)
    return (routed + shared).reshape(B, S, D)


def setup_inputs(seed: int = 0) -> dict:
    key = jax.random.key(seed)
    ks = jax.random.split(key, 32)
    f32 = jnp.float32

    def nrm(k, shape, s):
        return jax.random.normal(k, shape, f32) * s

    D = D_MODEL
    return {
        'x': nrm(ks[0], (BATCH, SEQ, D), 1.0),
        'c': nrm(ks[1], (BATCH, D), 1.0),
        'ada_w': nrm(ks[2], (DEPTH, D, 6 * D), 0.2 * D ** -0.5),
        'ada_b': nrm(ks[3], (DEPTH, 6 * D), 0.02),
        'norm_mix': 1.0 + nrm(ks[4], (DEPTH, D), 0.02),
        'norm_ffn': 1.0 + nrm(ks[5], (DEPTH, D), 0.02),
        'a_w_in': nrm(ks[6], (N_A_LAYERS, D, 2 * A_WIDTH), D ** -0.5),
        'a_b_in': nrm(ks[7], (N_A_LAYERS, 2 * A_WIDTH), 0.02),
        'a_ln_g': 1.0 + nrm(ks[8], (N_A_LAYERS, A_WIDTH), 0.02),
        'a_ln_b': nrm(ks[9], (N_A_LAYERS, A_WIDTH), 0.02),
        'a_w_s': nrm(ks[10], (N_A_LAYERS, A_GROUPS, A_CHUNK, A_CHUNK), 0.5 * A_CHUNK ** -0.5),
        'a_b_s': 1.0 + nrm(ks[11], (N_A_LAYERS, A_GROUPS, A_CHUNK), 0.02),
        'a_w_out': nrm(ks[12], (N_A_LAYERS, A_WIDTH, D), A_WIDTH ** -0.5),
        'kv_norm': 1.0 + nrm(ks[13], (D,), 0.02),
        'kv_ada_w': nrm(ks[14], (D, 2 * D), 0.2 * D ** -0.5),
        'kv_ada_b': nrm(ks[15], (2 * D,), 0.02),
        'kv_w_k': nrm(ks[16], (D, D), D ** -0.5),
        'kv_w_v': nrm(ks[17], (D, D), D ** -0.5),
        'b_w_q': nrm(ks[18], (N_B_LAYERS, D, D), D ** -0.5),
        'b_w_o': nrm(ks[19], (N_B_LAYERS, D, D), D ** -0.5),
        'moe_router': nrm(ks[20], (DEPTH, D, N_EXPERTS), D ** -0.5),
        'moe_bias': nrm(ks[21], (DEPTH, N_EXPERTS), 0.01),
        'moe_w_gate': nrm(ks[22], (DEPTH, N_EXPERTS, D, EXPERT_DIM), D ** -0.5),
        'moe_w_up': nrm(ks[23], (DEPTH, N_EXPERTS, D, EXPERT_DIM), D ** -0.5),
        'moe_w_down': nrm(ks[24], (DEPTH, N_EXPERTS, EXPERT_DIM, D), EXPERT_DIM ** -0.5),
        'sh_w_gate': nrm(ks[25], (DEPTH, D, SHARED_DIM), D ** -0.5),
        'sh_w_up': nrm(ks[26], (DEPTH, D, SHARED_DIM), D ** -0.5),
        'sh_w_down': nrm(ks[27], (DEPTH, SHARED_DIM, D), SHARED_DIM ** -0.5),
        'final_norm': 1.0 + nrm(ks[28], (D,), 0.02),
    }


def reference(x, c, ada_w, ada_b, norm_mix, norm_ffn, a_w_in, a_b_in, a_ln_g, a_ln_b,
              a_w_s, a_b_s, a_w_out, kv_norm, kv_ada_w, kv_ada_b, kv_w_k, kv_w_v,
              b_w_q, b_w_o, moe_router, moe_bias, moe_w_gate, moe_w_up, moe_w_down,
              sh_w_gate, sh_w_up, sh_w_down, final_norm):
    kv = None
    for i in range(DEPTH):
        sh1, sc1, g1, sh2, sc2, g2 = ada(c, ada_w[i], ada_b[i], 6)
        h = modulate(rmsnorm(x, norm_mix[i]), sh1, sc1)
        if i < N_A_LAYERS:
            y = gmlp_mixer(h, a_w_in[i], a_b_in[i], a_ln_g[i], a_ln_b[i],
                           a_w_s[i], a_b_s[i], a_w_out[i])
        else:
            if kv is None:
                kv = shared_kv(x, c, kv_norm, kv_ada_w, kv_ada_b, kv_w_k, kv_w_v)
            j = i - N_A_LAYERS
            y = moba_mixer(h, b_w_q[j], b_w_o[j], kv[0], kv[1], kv[2])
        x = x + g1 * y
        h = modulate(rmsnorm(x, norm_ffn[i]), sh2, sc2)
        x = x + g2 * moe(h, moe_router[i], moe_bias[i], moe_w_gate[i], moe_w_up[i],
                         moe_w_down[i], sh_w_gate[i], sh_w_up[i], sh_w_down[i])
    return rmsnorm(x, final_norm)
```

```python
import os
import numpy as np
import concourse.bass as bass
import concourse.mybir as mybir
from concourse.bass_utils import run_bass_kernel_spmd
from concourse.bass import IndirectOffsetOnAxis

F32 = mybir.dt.float32
BF16 = mybir.dt.bfloat16
I32 = mybir.dt.int32
U32 = mybir.dt.uint32
AF = mybir.ActivationFunctionType
ALU = mybir.AluOpType
AX = mybir.AxisListType

SAME_ENGINE_SYNC = True
NEG = -30000.0
ATT_STOP = int(os.environ.get('ATT_STOP', '9'))
KV_SKIP = int(os.environ.get('KV_SKIP', '0'))
GATE_SKIP = int(os.environ.get('GATE_SKIP', '0'))
SPARSE = int(os.environ.get('SPARSE', '1'))
CAP = 512


class Prog:
    ENGS = ("pe", "act", "dve", "pool", "sp")

    def __init__(self, nc):
        self.nc = nc
        self.streams = {e: [] for e in self.ENGS}
        self.ecount = {e: 0 for e in self.ENGS}
        self.waited = {e: {} for e in self.ENGS}
        self.last_w = {}
        self.readers = {}
        self.sems = {}
        self.dcount = {}
        self.nsem = 0
        self.nops = 0

    def sem(self, key):
        if key not in self.sems:
            self.sems[key] = self.nc.alloc_semaphore("s%d" % self.nsem)
            self.nsem += 1
        return self.sems[key]

    def op(self, eng, fn, reads=(), writes=(), dsem=None):
        waits = {}
        self.nops += 1

        def need(dep):
            if dep is None:
                return
            k, v = dep
            if k == eng and (eng == "pe" or not SAME_ENGINE_SYNC):
                return
            if self.waited[eng].get(k, 0) >= v:
                return
            if waits.get(k, 0) < v:
                waits[k] = v

        for r in reads:
            need(self.last_w.get(r))
        for w in writes:
            need(self.last_w.get(w))
            for d in self.readers.get(w, {}).items():
                need(d)
        for k, v in waits.items():
            self.waited[eng][k] = v
        if dsem is None:
            self.ecount[eng] += 1
            tok = (eng, self.ecount[eng])
        else:
            self.dcount[dsem] = self.dcount.get(dsem, 0) + 16
            tok = (dsem, self.dcount[dsem])
        for r in reads:
            d = self.readers.setdefault(r, {})
            if d.get(tok[0], 0) < tok[1]:
                d[tok[0]] = tok[1]
        for w in writes:
            self.last_w[w] = tok
            self.readers[w] = {}
        self.streams[eng].append((list(waits.items()), fn, tok))
        return tok

    def barrier(self):
        cur = dict(self.dcount)
        for e in self.ENGS:
            cur[e] = self.ecount[e]
        for e in self.ENGS:
            waits = []
            for k, v in cur.items():
                if k == e or v == 0:
                    continue
                if self.waited[e].get(k, 0) < v:
                    self.waited[e][k] = v
                    waits.append((k, v))
            if waits:
                self.streams[e].append((waits, None, None))
        for e in ("act", "dve", "pool"):
            v = self.ecount[e]
            if v and self.waited[e].get(e, 0) < v:
                self.waited[e][e] = v
                self.streams[e].append(([(e, v)], None, None))
        self.last_w = {}
        self.readers = {}

    def emit(self):
        nc = self.nc
        for e in self.ENGS:
            self.sem(e)

        def run(e, name):
            for waits, fn, tok in self.streams[name]:
                for k, v in waits:
                    e.wait_ge(self.sem(k), v)
                if fn is None:
                    continue
                ins = fn(e)
                ins.then_inc(self.sem(tok[0]), 1 if tok[0] in self.ENGS else 16)

        with nc.Block() as block:
            @block.tensor
            def _(e):
                run(e, "pe")

            @block.scalar
            def _(e):
                run(e, "act")

            @block.vector
            def _(e):
                run(e, "dve")

            @block.gpsimd
            def _(e):
                run(e, "pool")

            @block.sync
            def _(e):
                run(e, "sp")


NT = 16
D = 1024
KC = 8
S = 2048


def build(stop_after=99, dbg=False):
    nc = bass.Bass("TRN2", target_bir_lowering=False)

    def din(name, shape):
        return nc.dram_tensor(name, list(shape), F32, kind="ExternalInput").ap()

    x_d = din("x", [S, D])
    c_d = din("c", [128, 8])
    ada_w_d = din("ada_w", [2, D, 6 * D])
    ada_b_d = din("ada_b", [2, 6 * D])
    norms_d = din("norms", [6, D])
    a_w_in_d = din("a_w_in", [D, 4096])
    a_b_in_u_d = din("a_b_in_u", [128, 16])
    a_b_in_v_d = din("a_b_in_v", [1, 2048])
    a_ln_d = din("a_ln", [2, 2048])
    a_w_sT_d = din("a_w_sT", [128, 8, 128])
    a_b_s_d = din("a_b_s", [1, 1024])
    a_w_out_d = din("a_w_out", [2048, D])
    kv_ada_w_d = din("kv_ada_w", [D, 2 * D])
    kv_ada_b_d = din("kv_ada_b", [1, 2 * D])
    kv_w_k_d = din("kv_w_k", [D, D])
    kv_w_v_d = din("kv_w_v", [D, D])
    b_w_q_d = din("b_w_q", [D, D])
    b_w_o_d = din("b_w_o", [D, D])
    router_d = din("moe_router", [2, D, 64])
    mbias_d = din("moe_bias", [2, 64])
    wg_d = din("moe_w_gate", [2, 64, D, 256])
    wu_d = din("moe_w_up", [2, 64, D, 256])
    wd_d = din("moe_w_down", [2, 64, 256, D])
    swg_d = din("sh_w_gate", [2, D, 256])
    swu_d = din("sh_w_up", [2, D, 256])
    swd_d = din("sh_w_down", [2, 256, D])
    ident_d = din("ident", [128, 128])
    trimask_d = din("trimask", [128, 128])
    tribias_d = din("tribias", [128, 128])
    iota_d = din("iota512", [128, CAP])
    pidx_d = din("pidx", [128, 1])
    tix_d = din("tix", [128, NT * 64])
    ustrict_d = din("ustrict", [64, 64])
    lstrict_d = din("lstrict", [128, 128])
    out_d = nc.dram_tensor("out", [S, D], F32, kind="ExternalOutput").ap()
    h_dram = nc.dram_tensor("h_scr", [S, D], BF16, kind="Internal").ap()
    ybuf = nc.dram_tensor("y_scr", [S * 8, D], F32, kind="Internal").ap()
    wscr = nc.dram_tensor("w_scr", [12, 128, 8, 512], BF16, kind="Internal").ap()

    P = Prog(nc)
    breg = [None]
    greg = [None]

    x_tok = nc.alloc_sbuf_tensor("x_tok", [128, NT, D], F32)
    mod = nc.alloc_sbuf_tensor("mod", [128, 6, D], F32)
    ident_f = nc.alloc_sbuf_tensor("ident_f", [128, 128], F32)
    ident_b = nc.alloc_sbuf_tensor("ident_b", [128, 128], BF16)
    ones_f = nc.alloc_sbuf_tensor("ones_f", [128, 128], F32)
    ones_b = nc.alloc_sbuf_tensor("ones_b", [128, 128], BF16)
    tribias_b = nc.alloc_sbuf_tensor("tribias_b", [128, 128], BF16)
    c_col = nc.alloc_sbuf_tensor("c_col", [128, 8], F32)
    scb_b = nc.alloc_sbuf_tensor("scb_b", [128, 8, 128], BF16)
    stat = nc.alloc_sbuf_tensor("stat", [128, 64], F32)
    ARENA_BYTES = (nc.sbuf_bytes_remaining - 256) // 64 * 64
    arena = nc.alloc_sbuf_tensor("arena", [128, ARENA_BYTES // 4], F32)
    ps = [nc.alloc_psum_tensor("ps%d" % i, [128, 512], F32) for i in range(8)]

    class Arena:
        def __init__(self):
            self.off = 0

        def reset(self):
            self.off = 0

        def get(self, shape, dt):
            n = int(np.prod(shape[1:]))
            esz = 4 if dt == F32 else 2
            nb = (n * esz + 63) // 64 * 64
            off = self.off
            self.off += nb
            assert self.off <= ARENA_BYTES, (self.off, ARENA_BYTES)
            ap = arena[:, off // 4: off // 4 + nb // 4]
            if dt != F32:
                ap = ap.bitcast(dt)
            ap = ap[:, 0:n]
            if len(shape) == 3:
                ap = ap.rearrange("p (a b) -> p a b", a=shape[1])
            elif len(shape) == 4:
                ap = ap.rearrange("p (a b c) -> p a b c", a=shape[1], b=shape[2])
            return ap

    AR = Arena()

    def psb(i):
        return ps[i][:, :].bitcast(BF16)

    def dma(q, out, in_, writes=(), reads=(), key=None):
        return P.op(q, lambda e: e.dma_start(out=out, in_=in_), reads=list(reads), writes=list(writes), dsem=key)

    def pe(fns, reads, writes):
        def run(e):
            r = None
            for f in fns:
                r = f(e)
            return r
        return P.op("pe", run, reads=list(reads), writes=list(writes))

    def mm(out, lhsT, rhs, start, stop):
        return lambda e: e.matmul(out, lhsT=lhsT, rhs=rhs, start=start, stop=stop)

    def tr(out, in_, idn):
        return lambda e: e.transpose(out=out, in_=in_, identity=idn)

    def act(out, in_, func, reads, writes, **kw):
        return P.op("act", lambda e: e.activation(out=out, in_=in_, func=func, **kw), reads=list(reads), writes=list(writes))

    def dve(fn, reads, writes):
        return P.op("dve", fn, reads=list(reads), writes=list(writes))

    XR = lambda t: [("x", t, 0), ("x", t, 1)]

    dma("sp", ident_f[:], ident_d, writes=["ident_f"], key="c_i")
    dma("sp", c_col[:], c_d, writes=["c_col"], key="c_c")
    tb_f = AR.get([128, 128], F32)
    dma("sp", tb_f, tribias_d, writes=["tb_f"], key="c_t")
    dve(lambda e: e.tensor_copy(out=ident_b[:], in_=ident_f[:]), ["ident_f"], ["ident_b"])
    dve(lambda e: e.tensor_copy(out=tribias_b[:], in_=tb_f), ["tb_f"], ["tribias_b"])
    dve(lambda e: e.memset(ones_f[:], 1.0), [], ["ones_f"])
    dve(lambda e: e.memset(ones_b[:], 1.0), [], ["ones_b"])
    sc_col = stat[:, 56:64]
    act(sc_col, c_col[:], AF.Silu, ["c_col"], ["sc_col"])
    dve(lambda e: e.tensor_copy(out=scb_b[:], in_=sc_col.rearrange("p (k o) -> p k o", o=1).to_broadcast([128, 8, 128])),
        ["sc_col"], ["scb"])
    xv = x_d.rearrange("(t p) d -> p t d", p=128)
    for t in range(NT):
        dma("sp", x_tok[:, t, :], xv[:, t, :], writes=XR(t), key=("xl", t % 4))

    def mods(w_ap, b_ap, ncols, spec, base=0):
        P.barrier()
        AR.off = base
        CW = 512
        brow = [AR.get([128, CW], F32) for _ in range(2)]
        nrow = [AR.get([128, CW], F32) for _ in range(2)]
        ntmp = AR.get([128, CW], F32)
        NB_ = 3 if base == 0 else 2
        wbuf = [AR.get([128, 8, CW], BF16) for _ in range(NB_)]
        wv = w_ap.rearrange("(kc p) n -> p kc n", p=128)
        per = D // CW
        for j in range(ncols // CW):
            b = j % 2
            wb_ = j % NB_
            v, q = j // per, j % per
            slot, nr = spec[v]
            dma("sp", brow[b][0:1, :], b_ap[:, j * CW:(j + 1) * CW], writes=[("brow", b)], key=("m_b", b))
            dma("pool", wbuf[wb_], wv[:, :, j * CW:(j + 1) * CW], writes=[("wbuf", wb_)], key=("m_w", wb_))
            fns = [mm(ps[b][:, 0:CW], scb_b[:, kc, :], wbuf[wb_][:, kc, :], kc == 0, False) for kc in range(KC)]
            fns.append(mm(ps[b][:, 0:CW], ones_f[0:1, :], brow[b][0:1, :], False, True))
            pe(fns, [("wbuf", wb_), "scb", ("brow", b), "ones_f"], [("ps", b)])
            dst = mod[:, slot, q * CW:(q + 1) * CW]
            if nr is None:
                act(dst, ps[b][:, 0:CW], AF.Copy, [("ps", b)], [("mod", slot, (q * CW) // 512)])
            else:
                dma("sp", nrow[b][0:1, :], norms_d[nr:nr + 1, q * CW:(q + 1) * CW], writes=[("nrow", b)], key=("m_n", b))
                pe([mm(ps[2 + b][:, 0:CW], ones_f[0:1, :], nrow[b][0:1, :], True, True)],
                   [("nrow", b), "ones_f"], [("ps", 2 + b)])
                act(ntmp, ps[2 + b][:, 0:CW], AF.Copy, [("ps", 2 + b)], ["ntmp"])
                dve(lambda e, dst=dst, b=b: e.scalar_tensor_tensor(out=dst, in0=ps[b][:, 0:CW], scalar=1.0, in1=ntmp,
                                                                      op0=ALU.add, op1=ALU.mult),
                    [("ps", b), "ntmp"], [("mod", slot, (q * CW) // 512)])

    def bcast_row(row_ap, slot):
        P.barrier()
        AR.reset()
        nrow = AR.get([128, D], F32)
        dma("sp", nrow[0:1, :], row_ap, writes=["nrow"], key="m_r")
        for half in range(2):
            pe([mm(ps[half][:, :], ones_f[0:1, :], nrow[0:1, half * 512:(half + 1) * 512], True, True)],
               ["nrow", "ones_f"], [("ps", half)])
            act(mod[:, slot, half * 512:(half + 1) * 512], ps[half][:, :], AF.Copy, [("ps", half)], [("mod", slot, half)])

    MR = lambda s: [("mod", s, 0), ("mod", s, 1)]

    def norm_tile(t, sa, sb, hT, col0, hname, psbank, scr, hbname="hb", hdram=None):
        ss = stat[:, 0:1]
        rs = stat[:, 1:2]
        t1, hb = scr
        dve(lambda e: e.memset(ss, 0.0), [], ["ss"])
        act(hb, x_tok[:, t, :], AF.Square, XR(t) + ["ss"], [hbname, "ss"], accum_out=ss)
        act(rs, ss, AF.Sqrt, ["ss"], ["rs"], bias=1e-6, scale=1.0 / D)
        dve(lambda e: e.reciprocal(out=rs, in_=rs), ["rs"], ["rs"])
        dve(lambda e: e.scalar_tensor_tensor(out=t1, in0=x_tok[:, t, :], scalar=rs, in1=mod[:, sa, :], op0=ALU.mult, op1=ALU.mult),
            XR(t) + ["rs"] + MR(sa), ["t1"])
        dve(lambda e: e.tensor_tensor(out=hb, in0=t1, in1=mod[:, sb, :], op=ALU.add), ["t1"] + MR(sb), [hbname])
        if hdram is not None:
            dma("sp", hdram, hb, reads=[hbname], writes=[("hdram", t)], key=("hd", t % 2))
        pb = psb(psbank)
        pe([tr(pb[:, kc * 128:(kc + 1) * 128], hb[:, kc * 128:(kc + 1) * 128], ident_b[:]) for kc in range(KC)],
           [hbname, "ident_b"], [("ps", psbank)])
        act(hT[:, :, col0:col0 + 128], pb.rearrange("p (k c) -> p k c", k=KC), AF.Copy, [("ps", psbank)], [hname])

    def wload(dst, src, wname, key):
        dma("pool", dst, src, writes=[wname], key=key)

    def wload2(dst, src, wname, key):
        for hf in range(2):
            dma("pool", dst[:, :, hf * 512:(hf + 1) * 512], src[:, :, hf * 512:(hf + 1) * 512], writes=[(wname, hf)], key=(key, hf))

    def gmlp_phase():
        P.barrier()
        AR.reset()
        TG = 256
        NG = S // TG
        hT_G = AR.get([128, KC, TG], BF16)
        NCH = 4
        wch = [AR.get([128, 8, 512], BF16) for _ in range(NCH)]
        uT_G = AR.get([128, 16, TG], BF16)
        yT_G = AR.get([128, 16, TG], BF16)
        vf = AR.get([128, 2, 2048], F32)
        vn = AR.get([128, 2, 2048], BF16)
        lnG = AR.get([128, 2048], F32)
        lnB = AR.get([128, 2048], F32)
        WmT = AR.get([128, 8, 128], BF16)
        t1 = AR.get([128, D], F32)
        hb = AR.get([128, D], BF16)
        rows = vf[:, 0, :]
        biv_hi = AR.get([128, 2048], BF16)
        biv_lo = AR.get([128, 2048], BF16)
        bs_hi = AR.get([128, 1024], BF16)
        bs_lo = AR.get([128, 1024], BF16)
        biu = AR.get([128, 16], F32)
        tmpf = t1
        for which, dst in ((0, lnG), (1, lnB)):
            dma("sp", rows[0:1, :], a_ln_d[which:which + 1, :], writes=["rows"], key="g_r")
            for j in range(4):
                pe([mm(ps[j][:, :], ones_f[0:1, :], rows[0:1, j * 512:(j + 1) * 512], True, True)], ["rows", "ones_f"], [("ps", j)])
                act(dst[:, j * 512:(j + 1) * 512], ps[j][:, :], AF.Copy, [("ps", j)], [("ln", which)])
        dma("sp", rows[0:1, :], a_b_in_v_d, writes=["rows"], key="g_r")
        dve(lambda e: e.tensor_copy(out=biv_hi[0:1, :], in_=rows[0:1, :]), ["rows"], ["biv_hi"])
        dve(lambda e: e.tensor_tensor(out=rows[0:1, :], in0=rows[0:1, :], in1=biv_hi[0:1, :], op=ALU.subtract), ["rows", "biv_hi"], ["rows"])
        dve(lambda e: e.tensor_copy(out=biv_lo[0:1, :], in_=rows[0:1, :]), ["rows"], ["biv_lo"])
        dma("sp", rows[0:1, 0:1024], a_b_s_d, writes=["rows"], key="g_r")
        dve(lambda e: e.tensor_copy(out=bs_hi[0:1, :], in_=rows[0:1, 0:1024]), ["rows"], ["bs_hi"])
        dve(lambda e: e.tensor_tensor(out=rows[0:1, 0:1024], in0=rows[0:1, 0:1024], in1=bs_hi[0:1, :], op=ALU.subtract), ["rows", "bs_hi"], ["rows"])
        dve(lambda e: e.tensor_copy(out=bs_lo[0:1, :], in_=rows[0:1, 0:1024]), ["rows"], ["bs_lo"])
        dma("sp", biu, a_b_in_u_d, writes=["biu"], key="g_biu")
        wsf = tmpf.rearrange("p (g t) -> p g t", g=8)
        dma("sp", wsf, a_w_sT_d, writes=["t1"], key="g_ws")
        tmk = hb.bitcast(F32)[:, 0:128]
        dma("sp", tmk, trimask_d, writes=["tmk"], key="g_tm")
        dve(lambda e: e.tensor_tensor(out=WmT, in0=wsf, in1=tmk.rearrange("p (o t) -> p o t", o=1).to_broadcast([128, 8, 128]), op=ALU.mult),
            ["t1", "tmk"], ["WmT"])
        P.barrier()

        w_in_v = a_w_in_d.rearrange("(kc p) n -> p kc n", p=128)
        w_out_v = a_w_out_d.rearrange("(fc p) n -> p fc n", p=128)
        nch = [0]

        def next_chunk(src):
            b = nch[0] % NCH
            cid = nch[0] % 12
            first = nch[0] < 12
            nch[0] += 1
            if first:
                wload(wch[b], src, ("wch", b), ("g_w", b))
                dma("sp", wscr[cid], wch[b], reads=[("wch", b)], writes=[("wscr", cid)], key=("g_ws2", cid % 4))
            else:
                dma("sp", wch[b], wscr[cid], reads=[("wscr", cid)], writes=[("wch", b)], key=("g_w2", b))
            return b

        for G in range(NG):
            for tt in range(2):
                norm_tile(2 * G + tt, 0, 1, hT_G, tt * 128, "hT_G", 7, (t1, hb))
            for c in range(4):
                b = next_chunk(w_in_v[:, :, c * 512:(c + 1) * 512])
                for f in range(4):
                    fc = c * 4 + f
                    pb = fc % 2
                    pe([mm(ps[pb][:, 0:TG], wch[b][:, kc, f * 128:(f + 1) * 128], hT_G[:, kc, :], kc == 0, kc == KC - 1) for kc in range(KC)],
                       [("wch", b), "hT_G"], [("ps", pb)])
                    act(uT_G[:, fc, :], ps[pb][:, 0:TG], AF.Gelu, [("ps", pb), "biu"], [("uT", fc)], bias=biu[:, fc:fc + 1], scale=1.0)
            dve(lambda e: e.memset(stat[:, 8:16], 0.0), [], [("vs", tt, c) for tt in range(2) for c in range(4)])
            for c in range(4):
                b = next_chunk(w_in_v[:, :, 2048 + c * 512: 2048 + (c + 1) * 512])
                for tt in range(2):
                    pb = 2 + (c * 2 + tt) % 2
                    fns = [mm(ps[pb][:, :], hT_G[:, kc, tt * 128:(tt + 1) * 128], wch[b][:, kc, :], kc == 0, False) for kc in range(KC)]
                    fns.append(mm(ps[pb][:, :], ones_b[0:1, :], biv_hi[0:1, c * 512:(c + 1) * 512], False, False))
                    fns.append(mm(ps[pb][:, :], ones_b[0:1, :], biv_lo[0:1, c * 512:(c + 1) * 512], False, True))
                    pe(fns, [("wch", b), "hT_G", "ones_b", "biv_hi", "biv_lo"], [("ps", pb)])
                    act(vf[:, tt, c * 512:(c + 1) * 512], ps[pb][:, :], AF.Gelu, [("ps", pb)], [("vf", tt, c), ("vs", tt, c)],
                        accum_out=stat[:, 8 + tt * 4 + c: 9 + tt * 4 + c])
            def sc_(tt, k):
                return stat[:, 16 + tt * 8 + k: 17 + tt * 8 + k]
            TT = range(2)
            vts = [vf[:, tt, :] for tt in TT]
            vfrs = [[("vf", tt, c) for c in range(4)] for tt in TT]
            for tt in TT:
                s1, s2 = sc_(tt, 0), sc_(tt, 1)
                dve(lambda e, tt=tt, s1=s1: e.reduce_sum(out=s1, in_=stat[:, 8 + tt * 4: 12 + tt * 4], axis=AX.X), [("vs", tt, c) for c in range(4)], [("s1", tt)])
                dve(lambda e, s2=s2: e.memset(s2, 0.0), [], [("s2", tt)])
                act(vn[:, tt, :], vts[tt], AF.Square, vfrs[tt] + [("s2", tt)], [("vn", tt), ("s2", tt)], accum_out=s2)
            for tt in TT:
                s1, s2, mu, var = sc_(tt, 0), sc_(tt, 1), sc_(tt, 2), sc_(tt, 3)
                dve(lambda e, mu=mu, s1=s1: e.tensor_scalar(out=mu, in0=s1, scalar1=1.0 / 2048, scalar2=None, op0=ALU.mult), [("s1", tt)], [("mu", tt)])
                dve(lambda e, mu=mu, var=var: e.tensor_tensor(out=var, in0=mu, in1=mu, op=ALU.mult), [("mu", tt)], [("var", tt)])
                dve(lambda e, s2=s2, var=var: e.scalar_tensor_tensor(out=var, in0=s2, scalar=1.0 / 2048, in1=var, op0=ALU.mult, op1=ALU.subtract),
                    [("s2", tt), ("var", tt)], [("var", tt)])
            for tt in TT:
                act(sc_(tt, 4), sc_(tt, 3), AF.Sqrt, [("var", tt)], [("rstd", tt)], bias=1e-5, scale=1.0)
            for tt in TT:
                mu, rstd, nmr = sc_(tt, 2), sc_(tt, 4), sc_(tt, 5)
                dve(lambda e, rstd=rstd: e.reciprocal(out=rstd, in_=rstd), [("rstd", tt)], [("rstd", tt)])
                dve(lambda e, mu=mu, rstd=rstd, nmr=nmr: e.scalar_tensor_tensor(out=nmr, in0=mu, scalar=-1.0, in1=rstd, op0=ALU.mult, op1=ALU.mult),
                    [("mu", tt), ("rstd", tt)], [("nmr", tt)])
            for tt in TT:
                act(vts[tt], vts[tt], AF.Identity, vfrs[tt] + [("rstd", tt), ("nmr", tt)], vfrs[tt], bias=sc_(tt, 5), scale=sc_(tt, 4))
            for tt in TT:
                dve(lambda e, vt=vts[tt]: e.tensor_tensor(out=vt, in0=vt, in1=lnG, op=ALU.mult), vfrs[tt] + [("ln", 0)], vfrs[tt])
                dve(lambda e, vt=vts[tt], tt=tt: e.tensor_tensor(out=vn[:, tt, :], in0=vt, in1=lnB, op=ALU.add), vfrs[tt] + [("ln", 1)], [("vn", tt)])
            for tt in TT:
                for q4 in range(4):
                    bank = 4 + q4 if q4 < 3 else 0
                    fns = []
                    for f in range(4):
                        fc = q4 * 4 + f
                        g = fc // 2
                        o = ps[bank][:, f * 128:(f + 1) * 128]
                        fns.append(mm(o, vn[:, tt, fc * 128:(fc + 1) * 128], WmT[:, g, :], True, False))
                        fns.append(mm(o, ones_b[0:1, :], bs_hi[0:1, g * 128:(g + 1) * 128], False, False))
                        fns.append(mm(o, ones_b[0:1, :], bs_lo[0:1, g * 128:(g + 1) * 128], False, True))
                    pe(fns, [("vn", tt), "WmT", "ones_b", "bs_hi", "bs_lo"], [("ps", bank)])
                    dve(lambda e, bank=bank, q4=q4, tt=tt: e.tensor_tensor(
                        out=yT_G[:, q4 * 4:(q4 + 1) * 4, tt * 128:(tt + 1) * 128],
                        in0=ps[bank][:, :].rearrange("p (f t) -> p f t", f=4),
                        in1=uT_G[:, q4 * 4:(q4 + 1) * 4, tt * 128:(tt + 1) * 128], op=ALU.mult),
                        [("ps", bank)] + [("uT", q4 * 4 + f) for f in range(4)], [("yT", tt, q4)])
            for dh in range(2):
                for fh in range(2):
                    b = next_chunk(w_out_v[:, fh * 8:(fh + 1) * 8, dh * 512:(dh + 1) * 512])
                    for tt in range(2):
                        bank = 1 + tt
                        pe([mm(ps[bank][:, :], yT_G[:, fh * 8 + f, tt * 128:(tt + 1) * 128], wch[b][:, f, :], fh == 0 and f == 0, fh == 1 and f == 7)
                            for f in range(8)],
                           [("wch", b)] + [("yT", tt, q4) for q4 in range(4)], [("ps", bank)])
                for tt in range(2):
                    bank = 1 + tt
                    t = 2 * G + tt
                    tm = tmpf[:, 0:512]
                    dve(lambda e, bank=bank, dh=dh: e.tensor_tensor(out=tm, in0=ps[bank][:, :], in1=mod[:, 2, dh * 512:(dh + 1) * 512], op=ALU.mult),
                        [("ps", bank), ("mod", 2, dh)], ["t1"])
                    dve(lambda e, t=t, dh=dh: e.tensor_tensor(out=x_tok[:, t, dh * 512:(dh + 1) * 512], in0=x_tok[:, t, dh * 512:(dh + 1) * 512], in1=tm, op=ALU.add),
                        ["t1", ("x", t, dh)], [("x", t, dh)])

    def moe_phase(li):
        P.barrier()
        AR.reset()
        hT = AR.get([128, KC, S], BF16)
        NWB = 3
        wgb = [AR.get([128, 8, 256], BF16) for _ in range(NWB)]
        wub = [AR.get([128, 8, 256], BF16) for _ in range(NWB)]
        wdb = [AR.get([128, 2, D], BF16) for _ in range(NWB)]
        hid = [AR.get([128, 2, 512], BF16) for _ in range(2)]
        sg = [AR.get([128, 512], F32) for _ in range(2)]
        wr = AR.get([128, NT, 64], F32)
        wrb = AR.get([128, 8, 64], BF16)
        mbb = AR.get([128, 64], F32)
        t1 = AR.get([128, D], F32)
        hb = AR.get([128, D], BF16)
        rt = AR.get([128, 8, 64], F32)
        wload(wrb, router_d[li].rearrange("(kc p) n -> p kc n", p=128), "wrb", "r_w")
        dma("sp", t1[0:1, 0:64], mbias_d[li:li + 1, :], writes=["mrow"], key="r_b")
        pe([mm(ps[0][:, 0:64], ones_f[0:1, :], t1[0:1, 0:64], True, True)], ["mrow", "ones_f"], [("ps", 0)])
        act(mbb, ps[0][:, 0:64], AF.Copy, [("ps", 0)], ["mbb"])
        P.barrier()
        for t in range(NT):
            norm_tile(t, 3, 4, hT, t * 128, ("hT", t // 4), t % 2, (t1, hb))
        sc = rt[:, 0, :]; ch = rt[:, 1, :]; eq = rt[:, 2, :]; c2 = rt[:, 3, :]; cm = rt[:, 4, :]; wsel = rt[:, 5, :]
        m1 = rt[:, 6, 0:8]; m2 = rt[:, 6, 8:16]; gs = rt[:, 6, 16:24]; g8 = rt[:, 6, 24:32]; gm = rt[:, 6, 32:40]; e8 = rt[:, 6, 40:48]
        wsum = rt[:, 6, 48:49]
        g3 = lambda a: a.rearrange("p (g k) -> p g k", k=8)
        b3 = lambda a: a.rearrange("p (g o) -> p g o", o=1).to_broadcast([128, 8, 8])
        for t in range(NT):
            bank = 2 + t % 2
            pe([mm(ps[bank][:, 0:64], hT[:, kc, t * 128:(t + 1) * 128], wrb[:, kc, :], kc == 0, kc == KC - 1) for kc in range(KC)],
               [("hT", t // 4), "wrb"], [("ps", bank)])
            act(sc, ps[bank][:, 0:64], AF.Sigmoid, [("ps", bank)], ["sc"])
            dve(lambda e: e.tensor_tensor(out=ch, in0=sc, in1=mbb, op=ALU.add), ["sc", "mbb"], ["ch"])
            dve(lambda e: e.tensor_reduce(out=m1, in_=g3(ch), axis=AX.X, op=ALU.max), ["ch"], ["m1"])
            dve(lambda e: e.tensor_tensor(out=g3(eq), in0=g3(ch), in1=b3(m1), op=ALU.is_ge), ["ch", "m1"], ["eq"])
            dve(lambda e: e.scalar_tensor_tensor(out=c2, in0=eq, scalar=-1e30, in1=ch, op0=ALU.mult, op1=ALU.add), ["eq", "ch"], ["c2"])
            dve(lambda e: e.tensor_reduce(out=m2, in_=g3(c2), axis=AX.X, op=ALU.max), ["c2"], ["m2"])
            dve(lambda e: e.tensor_tensor(out=gs, in0=m1, in1=m2, op=ALU.add), ["m1", "m2"], ["gs"])
            dve(lambda e: e.max(out=g8, in_=gs), ["gs"], ["g8"])
            dve(lambda e: e.tensor_scalar(out=gm, in0=gs, scalar1=g8[:, 3:4], scalar2=None, op0=ALU.is_ge), ["gs", "g8"], ["gm"])
            dve(lambda e: e.scalar_tensor_tensor(out=g3(cm), in0=g3(ch), scalar=2.0, in1=b3(gm), op0=ALU.add, op1=ALU.mult), ["ch", "gm"], ["cm"])
            dve(lambda e: e.max(out=e8, in_=cm), ["cm"], ["e8"])
            dve(lambda e: e.scalar_tensor_tensor(out=wsel, in0=cm, scalar=e8[:, 7:8], in1=sc, op0=ALU.is_ge, op1=ALU.mult), ["cm", "e8", "sc"], ["wsel"])
            dve(lambda e: e.reduce_sum(out=wsum, in_=wsel, axis=AX.X), ["wsel"], ["wsum"])
            dve(lambda e: e.reciprocal(out=wsum, in_=wsum), ["wsum"], ["wsum"])
            dve(lambda e, t=t: e.tensor_scalar(out=wr[:, t, :], in0=wsel, scalar1=wsum, scalar2=2.5, op0=ALU.mult, op1=ALU.mult),
                ["wsel", "wsum"], [("wr", t)])
        G2 = 5
        for ei in range(65):
            b = ei % NWB
            if ei < 64:
                gsrc, usrc, dsrc = wg_d[li, ei], wu_d[li, ei], wd_d[li, ei]
            else:
                gsrc, usrc, dsrc = swg_d[li], swu_d[li], swd_d[li]
            wload(wgb[b], gsrc.rearrange("(kc p) n -> p kc n", p=128), ("wg", b), ("e_wg", b))
            wload(wub[b], usrc.rearrange("(kc p) n -> p kc n", p=128), ("wu", b), ("e_wu", b))
            wload(wdb[b], dsrc.rearrange("(fc p) n -> p fc n", p=128), ("wd", b), ("e_wd", b))
            dve(lambda e, b=b: e.tensor_tensor(out=wdb[b], in0=wdb[b], in1=mod[:, G2:G2 + 1, :].to_broadcast([128, 2, D]), op=ALU.mult),
                [("wd", b)] + MR(G2), [("wd", b)])
            for tg in range(4):
                hb_i = (ei * 4 + tg) % 2
                for f in range(2):
                    pe([mm(ps[f][:, :], wgb[b][:, kc, f * 128:(f + 1) * 128], hT[:, kc, tg * 512:(tg + 1) * 512], kc == 0, kc == KC - 1) for kc in range(KC)],
                       [("wg", b), ("hT", tg)], [("ps", f)])
                    pe([mm(ps[2 + f][:, :], wub[b][:, kc, f * 128:(f + 1) * 128], hT[:, kc, tg * 512:(tg + 1) * 512], kc == 0, kc == KC - 1) for kc in range(KC)],
                       [("wu", b), ("hT", tg)], [("ps", 2 + f)])
                    act(sg[f], ps[f][:, :], AF.Silu, [("ps", f)], [("sg", f)])
                    dve(lambda e, f=f, hb_i=hb_i: e.tensor_tensor(out=hid[hb_i][:, f, :], in0=sg[f], in1=ps[2 + f][:, :], op=ALU.mult),
                        [("sg", f), ("ps", 2 + f)], [("hid", hb_i, f)])
                for tt in range(4):
                    t = tg * 4 + tt
                    for dh in range(2):
                        bank = 4 + (tt * 2 + dh) % 4
                        pe([mm(ps[bank][:, :], hid[hb_i][:, f, tt * 128:(tt + 1) * 128], wdb[b][:, f, dh * 512:(dh + 1) * 512], f == 0, f == 1) for f in range(2)],
                           [("hid", hb_i, 0), ("hid", hb_i, 1), ("wd", b)], [("ps", bank)])
                        xs = x_tok[:, t, dh * 512:(dh + 1) * 512]
                        scal = wr[:, t, ei:ei + 1] if ei < 64 else 1.0
                        dve(lambda e, bank=bank, xs=xs, scal=scal: e.scalar_tensor_tensor(out=xs, in0=ps[bank][:, :], scalar=scal, in1=xs, op0=ALU.mult, op1=ALU.add),
                            [("ps", bank), ("x", t, dh), ("wr", t)], [("x", t, dh)])


    def moe_sparse(li):
        P.barrier()
        AR.reset()
        C = CAP
        NS = C // 128
        G2 = 5
        wr = AR.get([128, NT, 64], F32)
        posm = AR.get([128, NT, 64], F32)
        Rall = AR.get([128, NT * 64, 6], BF16)
        iotaC = AR.get([128, C], F32)
        persist = AR.off
        hT = AR.get([128, KC, S], BF16)
        mask_b = AR.get([128, NT, 64], BF16)
        wgb0 = AR.get([128, 8, 256], BF16)
        wub0 = AR.get([128, 8, 256], BF16)
        wdb0 = AR.get([128, 2, D], BF16)
        hid = [AR.get([128, 2, 512], BF16) for _ in range(2)]
        sg = [AR.get([128, 512], F32) for _ in range(2)]
        wrb = AR.get([128, 8, 64], BF16)
        mbb = AR.get([128, 64], F32)
        t1 = AR.get([128, D], F32)
        hbs = [AR.get([128, D], BF16) for _ in range(2)]
        R_sc = AR.get([128, NT * 64], F32)
        R_ch = AR.get([128, NT * 64], F32)
        R_t = AR.get([128, NT * 64], F32)
        tix = AR.get([128, NT * 64], F32)
        rm1 = AR.get([128, 128], F32)
        rm2 = AR.get([128, 128], F32)
        rgs = AR.get([128, 128], F32)
        rgm = AR.get([128, 128], F32)
        rg8 = AR.get([128, NT, 8], F32)
        re8 = AR.get([128, NT, 8], F32)
        rws = AR.get([128, NT], F32)
        U_b = AR.get([128, 64], BF16)
        L_b = AR.get([128, 128], BF16)
        maskT_all = AR.get([128, NT * 128], BF16)
        pidx = AR.get([128, 1], F32)
        dma("sp", iotaC, iota_d, writes=["iotaC"], key="k_io")
        dma("sp", pidx, pidx_d, writes=["pidx"], key="k_pi")
        dma("sp", tix, tix_d, writes=["tix"], key="k_ti")
        dma("pool", U_b[0:64, :], ustrict_d, writes=["U_b"], key="k_u")
        dma("pool", L_b, lstrict_d, writes=["L_b"], key="k_l")
        wload(wrb, router_d[li].rearrange("(kc p) n -> p kc n", p=128), "wrb", "r_w")
        dma("sp", t1[0:1, 0:64], mbias_d[li:li + 1, :], writes=["mrow"], key="r_b")
        pe([mm(ps[0][:, 0:64], ones_f[0:1, :], t1[0:1, 0:64], True, True)], ["mrow", "ones_f"], [("ps", 0)])
        act(mbb, ps[0][:, 0:64], AF.Copy, [("ps", 0)], ["mbb"])
        wload(wgb0, swg_d[li].rearrange("(kc p) n -> p kc n", p=128), "wg0", "e_wg0")
        wload(wub0, swu_d[li].rearrange("(kc p) n -> p kc n", p=128), "wu0", "e_wu0")
        wload(wdb0, swd_d[li].rearrange("(fc p) n -> p fc n", p=128), "wd0", "e_wd0")
        P.barrier()
        hv = h_dram.rearrange("(t p) d -> t p d", p=128)
        for t in range(NT):
            norm_tile(t, 3, 4, hT, t * 128, ("hT", t // 4), t % 2, (t1, hbs[t % 2]), hbname=("hb", t % 2), hdram=hv[t])
        v3 = lambda a_: a_.rearrange("p (g k) -> p g k", k=8)
        t3 = lambda a_: a_.rearrange("p (t e) -> p t e", e=64)
        bc = lambda a_, n: a_.rearrange("p (g o) -> p g o", o=1).to_broadcast([128, a_.shape[1], n])
        for t in range(NT):
            bank = 2 + t // 8
            pe([mm(ps[bank][:, (t % 8) * 64:(t % 8 + 1) * 64], hT[:, kc, t * 128:(t + 1) * 128], wrb[:, kc, :], kc == 0, kc == KC - 1) for kc in range(KC)],
               [("hT", t // 4), "wrb"], [("ps", bank)])
        for j in range(2):
            act(R_sc[:, j * 512:(j + 1) * 512], ps[2 + j][:, :], AF.Sigmoid, [("ps", 2 + j)], ["R_sc"])
        dve(lambda e: e.tensor_tensor(out=t3(R_ch), in0=t3(R_sc), in1=mbb.rearrange("p (o e) -> p o e", o=1).to_broadcast([128, NT, 64]), op=ALU.add),
            ["R_sc", "mbb"], ["R_ch"])
        dve(lambda e: e.tensor_reduce(out=rm1, in_=v3(R_ch), axis=AX.X, op=ALU.max), ["R_ch"], ["rm1"])
        dve(lambda e: e.tensor_tensor(out=v3(R_t), in0=v3(R_ch), in1=bc(rm1, 8), op=ALU.is_ge), ["R_ch", "rm1"], ["R_t"])
        dve(lambda e: e.scalar_tensor_tensor(out=R_t, in0=R_t, scalar=-1e30, in1=R_ch, op0=ALU.mult, op1=ALU.add), ["R_t", "R_ch"], ["R_t"])
        dve(lambda e: e.tensor_reduce(out=rm2, in_=v3(R_t), axis=AX.X, op=ALU.max), ["R_t"], ["rm2"])
        dve(lambda e: e.tensor_tensor(out=rgs, in0=rm1, in1=rm2, op=ALU.add), ["rm1", "rm2"], ["rgs"])
        rgs3 = rgs.rearrange("p (t g) -> p t g", g=8)
        for t in range(NT):
            dve(lambda e, t=t: e.max(out=rg8[:, t, :], in_=rgs3[:, t, :]), ["rgs"], ["rg8"])
        dve(lambda e: e.tensor_tensor(out=rgm.rearrange("p (t g) -> p t g", g=8), in0=rgs3, in1=rg8[:, :, 3:4].to_broadcast([128, NT, 8]), op=ALU.is_ge),
            ["rgs", "rg8"], ["rgm"])
        dve(lambda e: e.scalar_tensor_tensor(out=v3(R_t), in0=v3(R_ch), scalar=2.0, in1=bc(rgm, 8), op0=ALU.add, op1=ALU.mult), ["R_ch", "rgm"], ["R_t"])
        for t in range(NT):
            dve(lambda e, t=t: e.max(out=re8[:, t, :], in_=t3(R_t)[:, t, :]), ["R_t"], ["re8"])
        dve(lambda e: e.tensor_tensor(out=t3(R_ch), in0=t3(R_t), in1=re8[:, :, 7:8].to_broadcast([128, NT, 64]), op=ALU.is_ge), ["R_t", "re8"], ["R_ch"])
        dve(lambda e: e.tensor_copy(out=mask_b.rearrange("p t e -> p (t e)"), in_=R_ch), ["R_ch"], [("mask", t) for t in range(NT)])
        dve(lambda e: e.tensor_tensor(out=R_t, in0=R_ch, in1=R_sc, op=ALU.mult), ["R_ch", "R_sc"], ["R_t"])
        dve(lambda e: e.tensor_reduce(out=rws, in_=t3(R_t), axis=AX.X, op=ALU.add), ["R_t"], ["rws"])
        dve(lambda e: e.reciprocal(out=rws, in_=rws), ["rws"], ["rws"])
        dve(lambda e: e.scalar_tensor_tensor(out=wr, in0=t3(R_t), scalar=2.5, in1=bc(rws, 64), op0=ALU.mult, op1=ALU.mult),
            ["R_t", "rws"], [("wr", t) for t in range(NT)])
        dve(lambda e: e.tensor_tensor(out=wdb0, in0=wdb0, in1=mod[:, G2:G2 + 1, :].to_broadcast([128, 2, D]), op=ALU.mult), ["wd0"] + MR(G2), ["wd0"])
        for tg in range(4):
            hb_i = tg % 2
            for f in range(2):
                pe([mm(ps[f][:, :], wgb0[:, kc, f * 128:(f + 1) * 128], hT[:, kc, tg * 512:(tg + 1) * 512], kc == 0, kc == KC - 1) for kc in range(KC)],
                   ["wg0", ("hT", tg)], [("ps", f)])
                pe([mm(ps[2 + f][:, :], wub0[:, kc, f * 128:(f + 1) * 128], hT[:, kc, tg * 512:(tg + 1) * 512], kc == 0, kc == KC - 1) for kc in range(KC)],
                   ["wu0", ("hT", tg)], [("ps", 2 + f)])
                act(sg[f], ps[f][:, :], AF.Silu, [("ps", f)], [("sg", f)])
                dve(lambda e, f=f, hb_i=hb_i: e.tensor_tensor(out=hid[hb_i][:, f, :], in0=sg[f], in1=ps[2 + f][:, :], op=ALU.mult),
                    [("sg", f), ("ps", 2 + f)], [("hid", hb_i, f)])
            for tt in range(4):
                t = tg * 4 + tt
                for dh in range(2):
                    bank = 4 + (tt * 2 + dh) % 4
                    pe([mm(ps[bank][:, :], hid[hb_i][:, f, tt * 128:(tt + 1) * 128], wdb0[:, f, dh * 512:(dh + 1) * 512], f == 0, f == 1) for f in range(2)],
                       [("hid", hb_i, 0), ("hid", hb_i, 1), "wd0"], [("ps", bank)])
                    xs = x_tok[:, t, dh * 512:(dh + 1) * 512]
                    dve(lambda e, bank=bank, xs=xs: e.tensor_tensor(out=xs, in0=ps[bank][:, :], in1=xs, op=ALU.add),
                        [("ps", bank), ("x", t, dh)], [("x", t, dh)])
        mrd = [("mask", t) for t in range(NT)]
        for i in range(NT):
            bank = 4 + i // 8
            o = ps[bank][:, (i % 8) * 64:(i % 8 + 1) * 64]
            fns = [mm(o, ones_b[:], mask_b[:, j, :], j == 0, False) for j in range(i)]
            fns.append(mm(o, L_b, mask_b[:, i, :], i == 0, True))
            pe(fns, mrd + ["ones_b", "L_b"], [("ps", bank)])
        for j in range(2):
            pm = posm.rearrange("p t e -> p (t e)")[:, j * 512:(j + 1) * 512]
            dve(lambda e, pm=pm, j=j: e.scalar_tensor_tensor(out=pm, in0=ps[4 + j][:, :], scalar=1.0, in1=R_ch[:, j * 512:(j + 1) * 512], op0=ALU.add, op1=ALU.mult),
                [("ps", 4 + j), "R_ch"], [("posm", t) for t in range(NT)])
            dve(lambda e, pm=pm: e.tensor_scalar(out=pm, in0=pm, scalar1=-1.0, scalar2=None, op0=ALU.add),
                [("posm", t) for t in range(NT)], [("posm", t) for t in range(NT)])
        for j in range(2):
            pbk = psb(6 + j)
            pe([tr(pbk[0:64, (i % 8) * 128:(i % 8 + 1) * 128], mask_b[:, i, :], ident_b[:]) for i in range(j * 8, j * 8 + 8)],
               mrd + ["ident_b"], [("ps", 6 + j)])
            act(maskT_all[0:64, j * 1024:(j + 1) * 1024], pbk[0:64, :], AF.Copy, [("ps", 6 + j)], [("maskT_all", j)])
        for i in range(NT):
            bank = 2 + i // 8
            pe([mm(ps[bank][:, (i % 8) * 64:(i % 8 + 1) * 64], maskT_all[0:64, i * 128:(i + 1) * 128], U_b[0:64, :], True, True)],
               [("maskT_all", i // 8), "U_b"], [("ps", bank)])
        RR = [("R", i, c) for i in range(NT) for c in range(6)]
        c1 = lambda a_: a_.rearrange("p (a o) -> p a o", o=1)
        dve(lambda e: e.tensor_copy(out=Rall[:, :, 0:1], in_=c1(tix)), ["tix"], RR)
        dve(lambda e: e.tensor_copy(out=Rall[:, :, 1:2], in_=pidx.rearrange("p (a o) -> p a o", o=1).to_broadcast([128, NT * 64, 1])), ["pidx"], RR)
        dve(lambda e: e.memset(Rall[:, :, 2:3], 1.0), [], RR)
        for j in range(2):
            dve(lambda e, j=j: e.tensor_copy(out=Rall[:, j * 512:(j + 1) * 512, 3:4], in_=c1(ps[2 + j][:, :])), [("ps", 2 + j)], RR)
        wrf = wr.rearrange("p t e -> p (t e)")
        dve(lambda e: e.tensor_copy(out=Rall[:, :, 4:5], in_=c1(wrf)), [("wr", t) for t in range(NT)], RR)
        dve(lambda e: e.tensor_tensor(out=Rall[:, :, 5:6], in0=c1(wrf), in1=Rall[:, :, 4:5], op=ALU.subtract), [("wr", t) for t in range(NT)] + RR, RR)
        P.barrier()
        AR.off = persist
        NWB = 2
        wgb = [AR.get([128, 8, 256], BF16) for _ in range(NWB)]
        wub = [AR.get([128, 8, 256], BF16) for _ in range(NWB)]
        wdb = [AR.get([128, 2, D], BF16) for _ in range(NWB)]
        hg = [AR.get([128, NS, D], BF16) for _ in range(2)]
        hgT = [AR.get([128, KC, C], BF16) for _ in range(2)]
        hid2 = [AR.get([128, 2, C], BF16) for _ in range(2)]
        sg2 = [AR.get([128, C], BF16) for _ in range(2)]
        yout = AR.get([128, NS, D], F32)
        OH = [AR.get([128, C], BF16) for _ in range(8)]
        sl = [AR.get([128, NS, 8], F32) for _ in range(4)]
        sli = [AR.get([128, NS, 2], I32) for _ in range(4)]
        wsl = [AR.get([128, NS], F32) for _ in range(4)]
        NE = int(os.environ.get("NEXP", "64"))
        for hb_ in range(2):
            dve(lambda e, hb_=hb_: e.memset(hg[hb_].rearrange("p a b -> p (a b)"), 0.0), [], [("hg", hb_, s_) for s_ in range(NS)])

        def weights(ei):
            wb = ei % NWB
            wload(wgb[wb], wg_d[li, ei].rearrange("(kc p) n -> p kc n", p=128), ("wg", wb), ("e_wg", wb))
            wload(wub[wb], wu_d[li, ei].rearrange("(kc p) n -> p kc n", p=128), ("wu", wb), ("e_wu", wb))
            wload(wdb[wb], wd_d[li, ei].rearrange("(fc p) n -> p fc n", p=128), ("wd", wb), ("e_wd", wb))

        def g2fold(ei):
            wb = ei % NWB
            dve(lambda e, wb=wb: e.tensor_tensor(out=wdb[wb], in0=wdb[wb], in1=mod[:, G2:G2 + 1, :].to_broadcast([128, 2, D]), op=ALU.mult),
                [("wd", wb)] + MR(G2), [("wd", wb)])

        def ohs(ei, half):
            for i in range(half * 8, half * 8 + 8):
                o = i % 8
                dve(lambda e, o=o, i=i, ei=ei: e.tensor_scalar(out=OH[o], in0=iotaC, scalar1=posm[:, i, ei:ei + 1], scalar2=None, op0=ALU.is_equal),
                    ["iotaC", ("posm", i)], [("OH", o)])

        def pe_idx(ei, half):
            ib = 7
            fns = []
            for i in range(half * 8, half * 8 + 8):
                o = i % 8
                for s_ in range(NS):
                    fns.append(mm(ps[ib][:, s_ * 8:s_ * 8 + 6], OH[o][:, s_ * 128:(s_ + 1) * 128], Rall[:, i * 64 + ei, :],
                                  i == 0 and s_ == 0, i == NT - 1 and s_ == NS - 1))
            pe(fns, [("OH", o) for o in range(8)] + [("R", i, c) for i in range(half * 8, half * 8 + 8) for c in range(6)], [("ps", ib)])

        def slot_math(ei):
            b3_ = ei % 4
            ib = 7
            pv = ps[ib][:, 0:NS * 8].rearrange("p (s c) -> p s c", c=8)
            S_ = sl[b3_]
            rg = [("sl", b3_)]
            dve(lambda e: e.tensor_copy(out=S_[:, :, 0:6], in_=pv[:, :, 0:6]), [("ps", ib)], rg)
            dve(lambda e: e.scalar_tensor_tensor(out=S_[:, :, 6:7], in0=S_[:, :, 0:1], scalar=128.0, in1=S_[:, :, 1:2], op0=ALU.mult, op1=ALU.add), rg, rg)
            dve(lambda e: e.scalar_tensor_tensor(out=S_[:, :, 7:8], in0=S_[:, :, 6:7], scalar=8.0, in1=S_[:, :, 3:4], op0=ALU.mult, op1=ALU.add), rg, rg)
            dve(lambda e: e.scalar_tensor_tensor(out=S_[:, :, 7:8], in0=S_[:, :, 7:8], scalar=1.0, in1=S_[:, :, 2:3], op0=ALU.add, op1=ALU.mult), rg, rg)
            dve(lambda e: e.tensor_scalar(out=S_[:, :, 7:8], in0=S_[:, :, 7:8], scalar1=-1.0, scalar2=None, op0=ALU.add), rg, rg)
            dve(lambda e: e.scalar_tensor_tensor(out=S_[:, :, 6:7], in0=S_[:, :, 6:7], scalar=1.0, in1=S_[:, :, 2:3], op0=ALU.add, op1=ALU.mult), rg, rg)
            dve(lambda e: e.tensor_scalar(out=S_[:, :, 6:7], in0=S_[:, :, 6:7], scalar1=-1.0, scalar2=None, op0=ALU.add), rg, rg)
            dve(lambda e: e.tensor_copy(out=sli[b3_], in_=S_[:, :, 6:8]), rg, [("sli", b3_)])
            dve(lambda e: e.tensor_tensor(out=wsl[b3_].rearrange("p (s o) -> p s o", o=1), in0=S_[:, :, 4:5], in1=S_[:, :, 5:6], op=ALU.add),
                rg, [("wsl", b3_)])

        def gathers(ei):
            b = ei % 2
            b3_ = ei % 4
            for s_ in range(NS):
                def gath(e, b=b, s_=s_, b3_=b3_):
                    if greg[0] is None:
                        greg[0] = e.to_reg(S - 1)
                    return e.indirect_dma_start(out=hg[b][:, s_, :], out_offset=None, in_=h_dram,
                                                in_offset=IndirectOffsetOnAxis(ap=sli[b3_][:, s_, 0:1].bitcast(U32), axis=0),
                                                bounds_check=greg[0], oob_is_err=False)
                P.op("pool", gath, reads=[("sli", b3_)], writes=[("hg", b, s_)], dsem=("ga", b, s_))

        def T_(ei, half):
            b = ei % 2
            for s_ in range(half * 2, half * 2 + 2):
                bank = s_ % 2
                pb = psb(bank)
                pe([tr(pb[:, kc * 128:(kc + 1) * 128], hg[b][:, s_, kc * 128:(kc + 1) * 128], ident_b[:]) for kc in range(KC)],
                   [("hg", b, s_), "ident_b"], [("ps", bank)])
                act(hgT[b][:, :, s_ * 128:(s_ + 1) * 128], pb.rearrange("p (k c) -> p k c", k=KC), AF.Copy, [("ps", bank)], [("hgT", b, s_)])

        def GU_(ei, f):
            b = ei % 2
            wb = ei % NWB
            hgr = [("hgT", b, s_) for s_ in range(NS)]
            gb, ub = (2, 3) if f == 0 else (4, 5)
            pe([mm(ps[gb][:, 0:C], wgb[wb][:, kc, f * 128:(f + 1) * 128], hgT[b][:, kc, :], kc == 0, kc == KC - 1) for kc in range(KC)],
               [("wg", wb)] + hgr, [("ps", gb)])
            pe([mm(ps[ub][:, 0:C], wub[wb][:, kc, f * 128:(f + 1) * 128], hgT[b][:, kc, :], kc == 0, kc == KC - 1) for kc in range(KC)],
               [("wu", wb)] + hgr, [("ps", ub)])
            act(sg2[f], ps[gb][:, 0:C], AF.Silu, [("ps", gb)], [("sg2", f)])
            dve(lambda e, f=f, b=b, ub=ub: e.tensor_tensor(out=hid2[b][:, f, :], in0=sg2[f], in1=ps[ub][:, 0:C], op=ALU.mult),
                [("sg2", f), ("ps", ub)], [("hid2", b, f)])

        def B2(ei):
            b = ei % 2
            wb = ei % NWB
            b3_ = ei % 4
            for s_ in range(NS):
                for dh in range(2):
                    bank = 6 + dh
                    pe([mm(ps[bank][:, :], hid2[b][:, f, s_ * 128:(s_ + 1) * 128], wdb[wb][:, f, dh * 512:(dh + 1) * 512], f == 0, f == 1) for f in range(2)],
                       [("hid2", b, 0), ("hid2", b, 1), ("wd", wb)], [("ps", bank)])
                    act(yout[:, s_, dh * 512:(dh + 1) * 512], ps[bank][:, :], AF.Identity, [("ps", bank), ("wsl", b3_)], [("yout", s_)],
                        scale=wsl[b3_][:, s_:s_ + 1])

                def scat(e, s_=s_, b3_=b3_):
                    if breg[0] is None:
                        breg[0] = e.to_reg(S * 8 - 1)
                    return e.indirect_dma_start(out=ybuf, out_offset=IndirectOffsetOnAxis(ap=sli[b3_][:, s_, 1:2].bitcast(U32), axis=0),
                                                in_=yout[:, s_, :], in_offset=None, bounds_check=breg[0], oob_is_err=False)
                P.op("pool", scat, reads=[("yout", s_), ("sli", b3_)], writes=[], dsem=("sc", s_))

        def valid(e_):
            return 0 <= e_ < NE

        ohs(0, 0)
        for k in range(-3, NE):
            e3, e2, e1, e0 = k + 3, k + 2, k + 1, k
            if valid(e3):
                pe_idx(e3, 0)
                ohs(e3, 1)
            if valid(e2):
                T_(e2, 0)
            if valid(e1):
                GU_(e1, 0)
            if valid(e3):
                pe_idx(e3, 1)
                slot_math(e3)
                gathers(e3)
                if valid(e3 + 1):
                    ohs(e3 + 1, 0)
            if valid(e2):
                wb = e2 % NWB
                wload(wgb[wb], wg_d[li, e2].rearrange("(kc p) n -> p kc n", p=128), ("wg", wb), ("e_wg", wb))
                wload(wub[wb], wu_d[li, e2].rearrange("(kc p) n -> p kc n", p=128), ("wu", wb), ("e_wu", wb))
            if valid(e2):
                T_(e2, 1)
            if valid(e1):
                GU_(e1, 1)
            if valid(e0):
                B2(e0)
            if valid(e2):
                wb = e2 % NWB
                wload(wdb[wb], wd_d[li, e2].rearrange("(fc p) n -> p fc n", p=128), ("wd", wb), ("e_wd", wb))
        P.barrier()
        AR.reset()
        ybt = [AR.get([128, 8, D], F32) for _ in range(2)]
        accb = AR.get([128, D], F32)
        yv = ybuf.rearrange("(t p k) d -> t p k d", p=128, k=8)
        for t in range(NT):
            b = t % 2
            dma("sp", ybt[b], yv[t], writes=[("ybt", b)], key=("yl", b))
            dve(lambda e, b=b: e.tensor_tensor(out=accb, in0=ybt[b][:, 0, :], in1=ybt[b][:, 1, :], op=ALU.add), [("ybt", b)], ["accb"])
            for k in range(2, 8):
                dve(lambda e, b=b, k=k: e.tensor_tensor(out=accb, in0=accb, in1=ybt[b][:, k, :], op=ALU.add), [("ybt", b), "accb"], ["accb"])
            dve(lambda e: e.tensor_tensor(out=accb, in0=accb, in1=mod[:, G2, :], op=ALU.mult), ["accb"] + MR(G2), ["accb"])
            dve(lambda e, t=t: e.tensor_tensor(out=x_tok[:, t, :], in0=x_tok[:, t, :], in1=accb, op=ALU.add), ["accb"] + XR(t), XR(t))

    def attn_phase():
        P.barrier()
        AR.reset()
        TG = 256
        kT = AR.get([128, KC, S], BF16)
        v1 = AR.get([128, NT * 16, 66], BF16)
        wA = AR.get([128, 8, D], BF16)
        kmT = AR.get([128, 8, 8], BF16)
        kvmark = AR.off
        wB = AR.get([128, 8, D], BF16)
        hT_G = AR.get([128, KC, TG], BF16)
        t1 = AR.get([128, D], F32)
        hb = AR.get([128, D], BF16)
        kmf = AR.get([128, 8, 8], F32)
        dve(lambda e: e.memset(kmf, 0.0), [], ["kmf0"])
        if not (KV_SKIP & 1):
            dve(lambda e: e.memset(v1[:, :, 64:65], 1.0), [], ["v1ones"])
        wload2(wA, kv_w_k_d.rearrange("(kc p) n -> p kc n", p=128), "wA", "a_w")
        wload2(wB, kv_w_v_d.rearrange("(kc p) n -> p kc n", p=128), "wB", "a_w2")
        for G in range(S // TG):
            for tt in range(2):
                norm_tile(2 * G + tt, 0, 1, hT_G, tt * 128, "hT_G", 7, (t1, hb))
            for fc in range(8 if not (KV_SKIP & 2) else 0):
                pb = fc % 2
                pe([mm(ps[pb][:, 0:TG], wA[:, kc, fc * 128:(fc + 1) * 128], hT_G[:, kc, :], kc == 0, kc == KC - 1) for kc in range(KC)],
                   [("wA", fc // 4), "hT_G"], [("ps", pb)])
                act(kT[:, fc, G * TG:(G + 1) * TG], ps[pb][:, 0:TG], AF.Copy, [("ps", pb), "kmf0"], [("kT", G), ("kmf", G)],
                    accum_out=kmf[:, fc, G:G + 1])
            for tt in range(2 if not (KV_SKIP & 4) else 0):
                t = 2 * G + tt
                for dh in range(2):
                    bank = 2 + (tt * 2 + dh)
                    pe([mm(ps[bank][:, :], hT_G[:, kc, tt * 128:(tt + 1) * 128], wB[:, kc, dh * 512:(dh + 1) * 512], kc == 0, kc == KC - 1) for kc in range(KC)],
                       [("wB", dh), "hT_G"], [("ps", bank)])
                    act(v1[:, t * 16 + dh * 8: t * 16 + (dh + 1) * 8, 0:64], ps[bank][:, :].rearrange("p (h d) -> p h d", h=8), AF.Copy, [("ps", bank)], [("v1", t)])
        if not (KV_SKIP & 8):
            dve(lambda e: e.tensor_scalar(out=kmT, in0=kmf, scalar1=1.0 / 256, scalar2=None, op0=ALU.mult), [("kmf", G) for G in range(8)], ["kmT"])
        return kT, v1, wA, kvmark, kmT

    def attn_phase2(kT, v1, wA, kvmark, kmT):
        P.barrier()
        AR.off = kvmark
        TG = 256
        hT_G = AR.get([128, KC, TG], BF16)
        qT_G = AR.get([128, KC, 2, TG], BF16)
        o_tok = AR.get([128, 2, D], BF16)
        maskT = AR.get([128, TG], BF16)
        sel = [AR.get([128, 8, 128], BF16) for _ in range(2)]
        pT = [AR.get([128, TG], BF16) for _ in range(5)]
        g0 = AR.get([128, 16, 8], F32)
        g1 = AR.get([128, 16, 8], F32)
        eqt = AR.get([128, 16, 8], F32)
        mx = AR.get([128, 16], F32)
        bmb = AR.get([128, 128], BF16)
        rden = AR.get([128, 2], F32)
        t1 = AR.get([128, D], F32)
        hb = AR.get([128, D], BF16)
        tmpf = t1[:, 0:512]
        dve(lambda e: e.memset(qT_G.rearrange("p a b c -> p (a b c)"), 0.0), [], ["qT_G"])
        wqv = b_w_q_d.rearrange("(kc p) n -> p kc n", p=128)
        wov = b_w_o_d.rearrange("(kc p) n -> p kc n", p=128)
        npt = [0]
        bm3 = lambda a: a.rearrange("p (h o) -> p h o", o=1).to_broadcast([128, 16, 8])
        for G in range(S // TG):
            wload2(wA, wqv, "wA", "a_w")
            for tt in range(2):
                norm_tile(2 * G + tt, 0, 1, hT_G, tt * 128, "hT_G", 7, (t1, hb))
            for fc in range(8):
                pb = fc % 2
                pe([mm(ps[pb][:, 0:TG], wA[:, kc, fc * 128:(fc + 1) * 128], hT_G[:, kc, :], kc == 0, kc == KC - 1) for kc in range(KC)],
                   [("wA", fc // 4), "hT_G"], [("ps", pb)])
                act(qT_G[0:64, fc, 0, :], ps[pb][0:64, 0:TG], AF.Copy, [("ps", pb)], ["qT_G"])
                act(qT_G[64:128, fc, 1, :], ps[pb][64:128, 0:TG], AF.Copy, [("ps", pb)], ["qT_G"])
            wload2(wA, wov, "wA", "a_w")
            if ATT_STOP == 1:
                continue
            use_mask = G >= 4
            if use_mask:
                for tt in range(2):
                    if not (GATE_SKIP & 1):
                        pe([mm(ps[2][:, h * 8:(h + 1) * 8], qT_G[:, h // 2, h % 2, tt * 128:(tt + 1) * 128],
                               kmT[:, h // 2, :], True, True) for h in range(16)],
                           ["qT_G", "kmT"], [("ps", 2)])
                    if GATE_SKIP & 2:
                        continue
                    dve(lambda e: e.memset(g0, -1e30), [], ["g0"])
                    dve(lambda e, G=G: e.tensor_copy(out=g0[:, :, 0:G], in_=ps[2][:, 0:128].rearrange("p (h n) -> p h n", n=8)[:, :, 0:G]),
                        [("ps", 2), "g0"], ["g0"])
                    dve(lambda e: e.tensor_reduce(out=mx, in_=g0, axis=AX.X, op=ALU.max), ["g0"], ["mx"])
                    dve(lambda e: e.tensor_tensor(out=eqt, in0=g0, in1=bm3(mx), op=ALU.is_ge), ["g0", "mx"], ["eqt"])
                    dve(lambda e: e.scalar_tensor_tensor(out=g1, in0=eqt, scalar=-1e30, in1=g0, op0=ALU.mult, op1=ALU.add), ["eqt", "g0"], ["g1"])
                    dve(lambda e: e.tensor_reduce(out=mx, in_=g1, axis=AX.X, op=ALU.max), ["g1"], ["mx"])
                    dve(lambda e: e.tensor_tensor(out=eqt, in0=g1, in1=bm3(mx), op=ALU.is_ge), ["g1", "mx"], ["eqt"])
                    dve(lambda e: e.scalar_tensor_tensor(out=g1, in0=eqt, scalar=-1e30, in1=g1, op0=ALU.mult, op1=ALU.add), ["eqt", "g1"], ["g1"])
                    dve(lambda e: e.tensor_reduce(out=mx, in_=g1, axis=AX.X, op=ALU.max), ["g1"], ["mx"])
                    dve(lambda e: e.tensor_tensor(out=eqt, in0=g0, in1=bm3(mx), op=ALU.is_ge), ["g0", "mx"], ["eqt"])
                    dve(lambda e: e.tensor_scalar(out=bmb, in0=eqt.rearrange("p h n -> p (h n)"), scalar1=-1.0, scalar2=-NEG, op0=ALU.add, op1=ALU.mult),
                        ["eqt"], ["bmb"])
                    if GATE_SKIP & 4:
                        continue
                    pe([mm(ps[3][:, 0:128], bmb, ident_b[:], True, True)], ["bmb", "ident_b"], [("ps", 3)])
                    if not (GATE_SKIP & 8):
                        act(maskT[:, tt * 128:(tt + 1) * 128], ps[3][:, 0:128], AF.Copy, [("ps", 3)], ["maskT"])
            njt = 2 * G + 2
            items = [(h, jt) for h in range(16) for jt in range(njt)]

            def mk_sel(h):
                if use_mask:
                    dve(lambda e, h=h: e.tensor_copy(out=sel[h % 2], in_=ident_b[:, h * 8:(h + 1) * 8].rearrange("p (n o) -> p n o", o=1).to_broadcast([128, 8, 128])),
                        ["ident_b"], [("sel", h % 2)])

            def geom(jt, ii):
                qoff = 128 if jt == 2 * G + 1 else 0
                slot = ii % 4
                return qoff, slot, 0

            def S_(h, jt, ii):
                hp, hc = h % 2, h // 2
                qoff, sb, co = geom(jt, ii)
                st = ps[sb][:, co + qoff:co + TG]
                masked = use_mask and jt < 2 * G
                diag = jt >= 2 * G
                fns = [mm(st, kT[:, hc, jt * 128:(jt + 1) * 128], qT_G[:, hc, hp, qoff:TG], True, not (masked or diag))]
                rd = [("kT", jt // 2), "qT_G"]
                if masked:
                    fns.append(mm(st, sel[h % 2][:, jt // 2, :], maskT[:, qoff:TG], False, True))
                    rd += [("sel", h % 2), "maskT"]
                if diag:
                    fns.append(mm(ps[sb][:, co + qoff:co + qoff + 128], ident_b[:], tribias_b[:], False, True))
                    rd += ["ident_b", "tribias_b"]
                pe(fns, rd, [("ps", sb)])

            def EV_(h, jt, ii):
                qoff, sb, co = geom(jt, ii)
                st = ps[sb][:, co + qoff:co + TG]
                obs = [4 + (h % 2) * 2 + qt for qt in range(2)]
                oaccs = [ps[ob_][:, 0:65] for ob_ in obs]
                pi = npt[0] % 5
                npt[0] += 1
                act(pT[pi][:, qoff:TG], st, AF.Exp, [("ps", sb)], [("pT", pi)], scale=0.125)
                fns = []
                for qt in range(qoff // 128, 2):
                    last = (jt == 2 * G + qt)
                    fns.append(mm(oaccs[qt], pT[pi][:, qt * 128:(qt + 1) * 128], v1[:, jt * 16 + h, 0:65], jt == 0, last))
                pe(fns, [("pT", pi), ("v1", jt), "v1ones"], [("ps", obs[qt]) for qt in range(qoff // 128, 2)])
                if jt == njt - 1:
                    for qt in range(2):
                        dve(lambda e, qt=qt, oaccs=oaccs: e.reciprocal(out=rden[:, qt:qt + 1], in_=oaccs[qt][:, 64:65]), [("ps", obs[qt])], [("rden", qt)])
                        dve(lambda e, oaccs=oaccs, qt=qt, h=h: e.tensor_scalar(out=o_tok[:, qt, h * 64:(h + 1) * 64], in0=oaccs[qt][:, 0:64],
                                                                               scalar1=rden[:, qt:qt + 1], scalar2=None, op0=ALU.mult),
                            [("ps", obs[qt]), ("rden", qt)], [("o_tok", qt)])

            LA = 3
            mk_sel(0)
            mk_sel(1)
            for i0 in range(min(LA, len(items))):
                S_(items[i0][0], items[i0][1], i0)
            for ii, (h, jt) in enumerate(items):
                if ii + LA < len(items):
                    hn, jn = items[ii + LA]
                    if jn == 0 and hn + 1 < 16:
                        mk_sel(hn + 1)
                    S_(hn, jn, ii + LA)
                EV_(h, jt, ii)
            for tt in range(2):
                pbk = psb(6 + tt)
                pe([tr(pbk[:, kc * 128:(kc + 1) * 128], o_tok[:, tt, kc * 128:(kc + 1) * 128], ident_b[:]) for kc in range(KC)],
                   [("o_tok", tt), "ident_b"], [("ps", 6 + tt)])
                act(hT_G[:, :, tt * 128:(tt + 1) * 128], pbk.rearrange("p (k c) -> p k c", k=KC), AF.Copy, [("ps", 6 + tt)], ["hT_G"])
            for tt in range(2):
                t = 2 * G + tt
                for dh in range(2):
                    bank = 2 + dh
                    pe([mm(ps[bank][:, :], hT_G[:, kc, tt * 128:(tt + 1) * 128], wA[:, kc, dh * 512:(dh + 1) * 512], kc == 0, kc == KC - 1) for kc in range(KC)],
                       [("wA", dh), "hT_G"], [("ps", bank)])
                    dve(lambda e, bank=bank, dh=dh: e.tensor_tensor(out=tmpf, in0=ps[bank][:, :], in1=mod[:, 2, dh * 512:(dh + 1) * 512], op=ALU.mult),
                        [("ps", bank), ("mod", 2, dh)], ["t1"])
                    dve(lambda e, t=t, dh=dh: e.tensor_tensor(out=x_tok[:, t, dh * 512:(dh + 1) * 512], in0=x_tok[:, t, dh * 512:(dh + 1) * 512], in1=tmpf, op=ALU.add),
                        ["t1", ("x", t, dh)], [("x", t, dh)])

    def final_phase(raw):
        P.barrier()
        AR.reset()
        ob = [AR.get([128, D], F32) for _ in range(2)]
        ov = out_d.rearrange("(t p) d -> p t d", p=128)
        toks = []
        for t in range(NT):
            if raw:
                toks.append(dma("sp", ov[:, t, :], x_tok[:, t, :], reads=XR(t), key="st"))
                continue
            b = t % 2
            ss = stat[:, 0:1]; rs = stat[:, 1:2]
            dve(lambda e: e.memset(ss, 0.0), [], ["ss"])
            act(ob[b], x_tok[:, t, :], AF.Square, XR(t) + ["ss"], [("ob", b), "ss"], accum_out=ss)
            act(rs, ss, AF.Sqrt, ["ss"], ["rs"], bias=1e-6, scale=1.0 / D)
            dve(lambda e: e.reciprocal(out=rs, in_=rs), ["rs"], ["rs"])
            dve(lambda e, t=t, b=b: e.scalar_tensor_tensor(out=ob[b], in0=x_tok[:, t, :], scalar=rs, in1=mod[:, 0, :], op0=ALU.mult, op1=ALU.mult),
                XR(t) + ["rs"] + MR(0), [("ob", b)])
            toks.append(dma("sp", ov[:, t, :], ob[b], reads=[("ob", b)], key=("st", b)))
        P.barrier()

    L0 = [(1, None), (0, 0), (2, None), (4, None), (3, 1), (5, None)]
    L1 = [(1, None), (0, 2), (2, None), (4, None), (3, 3), (5, None)]
    steps = 0
    mods(ada_w_d[0], ada_b_d[0:1, :], 6 * D, L0)
    if stop_after >= 1:
        gmlp_phase()
    if stop_after >= 2:
        (moe_sparse if SPARSE else moe_phase)(0)
    if stop_after >= 3:
        mods(kv_ada_w_d, kv_ada_b_d, 2 * D, [(1, None), (0, 4)])
        if ATT_STOP == -1:
            final_phase(True)
            P.emit()
            return nc, P
        kvstate = attn_phase()
        if ATT_STOP == -2:
            final_phase(True)
            P.emit()
            return nc, P
        keep = kvstate[3]

        mods(ada_w_d[1], ada_b_d[1:2, :], 6 * D, L1, base=keep)
        if ATT_STOP != 0:
            attn_phase2(*kvstate)
    if stop_after >= 4:
        (moe_sparse if SPARSE else moe_phase)(1)
    if stop_after >= 5:
        bcast_row(norms_d[5:6, :], 0)
        final_phase(False)
    else:
        final_phase(True)
    P.emit()
    return nc, P


def host_inputs(inp):
    f = lambda a: np.ascontiguousarray(np.asarray(a, dtype=np.float32))
    sh = {}
    sh["ada_w"] = f(inp["ada_w"])
    sh["ada_b"] = f(inp["ada_b"])
    sh["norms"] = f(np.stack([inp["norm_mix"][0], inp["norm_ffn"][0], inp["norm_mix"][1], inp["norm_ffn"][1],
                              inp["kv_norm"], inp["final_norm"]], axis=0))
    sh["a_w_in"] = f(inp["a_w_in"][0])
    b_in = np.asarray(inp["a_b_in"][0], dtype=np.float32)
    sh["a_b_in_u"] = f(b_in[:2048].reshape(16, 128).T)
    sh["a_b_in_v"] = f(b_in[2048:].reshape(1, 2048))
    sh["a_ln"] = f(np.stack([inp["a_ln_g"][0], inp["a_ln_b"][0]], axis=0))
    sh["a_w_sT"] = f(np.transpose(np.asarray(inp["a_w_s"][0]), (2, 0, 1)))
    sh["a_b_s"] = f(np.asarray(inp["a_b_s"][0]).reshape(1, 1024))
    sh["a_w_out"] = f(inp["a_w_out"][0])
    sh["kv_ada_w"] = f(inp["kv_ada_w"])
    sh["kv_ada_b"] = f(np.asarray(inp["kv_ada_b"]).reshape(1, 2048))
    sh["kv_w_k"] = f(inp["kv_w_k"])
    sh["kv_w_v"] = f(inp["kv_w_v"])
    sh["b_w_q"] = f(inp["b_w_q"][0])
    sh["b_w_o"] = f(inp["b_w_o"][0])
    sh["moe_router"] = f(inp["moe_router"])
    sh["moe_bias"] = f(inp["moe_bias"])
    sh["moe_w_gate"] = f(inp["moe_w_gate"])
    sh["moe_w_up"] = f(inp["moe_w_up"])
    sh["moe_w_down"] = f(inp["moe_w_down"])
    sh["sh_w_gate"] = f(inp["sh_w_gate"])
    sh["sh_w_up"] = f(inp["sh_w_up"])
    sh["sh_w_down"] = f(inp["sh_w_down"])
    sh["ident"] = np.eye(128, dtype=np.float32)
    s_idx = np.arange(128)[:, None]
    t_idx = np.arange(128)[None, :]
    sh["trimask"] = (s_idx <= t_idx).astype(np.float32)
    sh["tribias"] = np.where(t_idx >= s_idx, 0.0, NEG).astype(np.float32)
    sh["iota512"] = np.ascontiguousarray(np.broadcast_to(np.arange(CAP, dtype=np.float32)[None, :], (128, CAP)))
    sh["pidx"] = np.arange(128, dtype=np.float32).reshape(128, 1)
    sh["tix"] = np.ascontiguousarray(np.broadcast_to(np.repeat(np.arange(NT, dtype=np.float32), 64)[None, :], (128, NT * 64)))
    sh["ustrict"] = (np.arange(64)[:, None] < np.arange(64)[None, :]).astype(np.float32)
    sh["lstrict"] = (s_idx < t_idx).astype(np.float32)
    return sh


def kernel(**inp):
    n = 8
    sh = host_inputs(inp)
    x = np.asarray(inp["x"], dtype=np.float32)
    c = np.asarray(inp["c"], dtype=np.float32)
    nc, _ = build()
    in_maps = []
    for b in range(n):
        m = dict(sh)
        m["x"] = np.ascontiguousarray(x[b])
        m["c"] = np.ascontiguousarray(c[b].reshape(8, 128).T)
        in_maps.append(m)
    res = run_bass_kernel_spmd(nc, in_maps, core_ids=list(range(n)))
    return np.stack([np.asarray(r["out"], dtype=np.float32) for r in res.results], axis=0)
```

```python
import os
import numpy as np
import concourse.bass as bass
import concourse.mybir as mybir
from concourse.bass_utils import run_bass_kernel_spmd
from concourse.bass import IndirectOffsetOnAxis

F32 = mybir.dt.float32
BF16 = mybir.dt.bfloat16
I32 = mybir.dt.int32
U32 = mybir.dt.uint32
AF = mybir.ActivationFunctionType
ALU = mybir.AluOpType
AX = mybir.AxisListType

SAME_ENGINE_SYNC = True
NEG = -30000.0
ATT_STOP = int(os.environ.get('ATT_STOP', '9'))
KV_SKIP = int(os.environ.get('KV_SKIP', '0'))
GATE_SKIP = int(os.environ.get('GATE_SKIP', '0'))
SPARSE = int(os.environ.get('SPARSE', '1'))
CAP = 512


class Prog:
    ENGS = ("pe", "act", "dve", "pool", "sp")

    def __init__(self, nc):
        self.nc = nc
        self.streams = {e: [] for e in self.ENGS}
        self.ecount = {e: 0 for e in self.ENGS}
        self.waited = {e: {} for e in self.ENGS}
        self.last_w = {}
        self.readers = {}
        self.sems = {}
        self.dcount = {}
        self.nsem = 0
        self.nops = 0

    def sem(self, key):
        if key not in self.sems:
            self.sems[key] = self.nc.alloc_semaphore("s%d" % self.nsem)
            self.nsem += 1
        return self.sems[key]

    def op(self, eng, fn, reads=(), writes=(), dsem=None):
        waits = {}
        self.nops += 1

        def need(dep):
            if dep is None:
                return
            k, v = dep
            if k == eng and (eng == "pe" or not SAME_ENGINE_SYNC):
                return
            if self.waited[eng].get(k, 0) >= v:
                return
            if waits.get(k, 0) < v:
                waits[k] = v

        for r in reads:
            need(self.last_w.get(r))
        for w in writes:
            need(self.last_w.get(w))
            for d in self.readers.get(w, {}).items():
                need(d)
        for k, v in waits.items():
            self.waited[eng][k] = v
        if dsem is None:
            self.ecount[eng] += 1
            tok = (eng, self.ecount[eng])
        else:
            self.dcount[dsem] = self.dcount.get(dsem, 0) + 16
            tok = (dsem, self.dcount[dsem])
        for r in reads:
            d = self.readers.setdefault(r, {})
            if d.get(tok[0], 0) < tok[1]:
                d[tok[0]] = tok[1]
        for w in writes:
            self.last_w[w] = tok
            self.readers[w] = {}
        self.streams[eng].append((list(waits.items()), fn, tok))
        return tok

    def barrier(self):
        cur = dict(self.dcount)
        for e in self.ENGS:
            cur[e] = self.ecount[e]
        for e in self.ENGS:
            waits = []
            for k, v in cur.items():
                if k == e or v == 0:
                    continue
                if self.waited[e].get(k, 0) < v:
                    self.waited[e][k] = v
                    waits.append((k, v))
            if waits:
                self.streams[e].append((waits, None, None))
        for e in ("act", "dve", "pool"):
            v = self.ecount[e]
            if v and self.waited[e].get(e, 0) < v:
                self.waited[e][e] = v
                self.streams[e].append(([(e, v)], None, None))
        self.last_w = {}
        self.readers = {}

    def emit(self):
        nc = self.nc
        for e in self.ENGS:
            self.sem(e)

        def run(e, name):
            for waits, fn, tok in self.streams[name]:
                for k, v in waits:
                    e.wait_ge(self.sem(k), v)
                if fn is None:
                    continue
                ins = fn(e)
                ins.then_inc(self.sem(tok[0]), 1 if tok[0] in self.ENGS else 16)

        with nc.Block() as block:
            @block.tensor
            def _(e):
                run(e, "pe")

            @block.scalar
            def _(e):
                run(e, "act")

            @block.vector
            def _(e):
                run(e, "dve")

            @block.gpsimd
            def _(e):
                run(e, "pool")

            @block.sync
            def _(e):
                run(e, "sp")


NT = 16
D = 1024
KC = 8
S = 2048


def build(stop_after=99, dbg=False):
    nc = bass.Bass("TRN2", target_bir_lowering=False)

    def din(name, shape):
        return nc.dram_tensor(name, list(shape), F32, kind="ExternalInput").ap()

    x_d = din("x", [S, D])
    c_d = din("c", [128, 8])
    ada_w_d = din("ada_w", [2, D, 6 * D])
    ada_b_d = din("ada_b", [2, 6 * D])
    norms_d = din("norms", [6, D])
    a_w_in_d = din("a_w_in", [D, 4096])
    a_b_in_u_d = din("a_b_in_u", [128, 16])
    a_b_in_v_d = din("a_b_in_v", [1, 2048])
    a_ln_d = din("a_ln", [2, 2048])
    a_w_sT_d = din("a_w_sT", [128, 8, 128])
    a_b_s_d = din("a_b_s", [1, 1024])
    a_w_out_d = din("a_w_out", [2048, D])
    kv_ada_w_d = din("kv_ada_w", [D, 2 * D])
    kv_ada_b_d = din("kv_ada_b", [1, 2 * D])
    kv_w_k_d = din("kv_w_k", [D, D])
    kv_w_v_d = din("kv_w_v", [D, D])
    b_w_q_d = din("b_w_q", [D, D])
    b_w_o_d = din("b_w_o", [D, D])
    router_d = din("moe_router", [2, D, 64])
    mbias_d = din("moe_bias", [2, 64])
    wg_d = din("moe_w_gate", [2, 64, D, 256])
    wu_d = din("moe_w_up", [2, 64, D, 256])
    wd_d = din("moe_w_down", [2, 64, 256, D])
    swg_d = din("sh_w_gate", [2, D, 256])
    swu_d = din("sh_w_up", [2, D, 256])
    swd_d = din("sh_w_down", [2, 256, D])
    ident_d = din("ident", [128, 128])
    trimask_d = din("trimask", [128, 128])
    tribias_d = din("tribias", [128, 128])
    iota_d = din("iota512", [128, CAP])
    pidx_d = din("pidx", [128, 1])
    tix_d = din("tix", [128, NT * 64])
    ustrict_d = din("ustrict", [64, 64])
    lstrict_d = din("lstrict", [128, 128])
    out_d = nc.dram_tensor("out", [S, D], F32, kind="ExternalOutput").ap()
    h_dram = nc.dram_tensor("h_scr", [S, D], BF16, kind="Internal").ap()
    ybuf = nc.dram_tensor("y_scr", [S * 8, D], F32, kind="Internal").ap()
    wscr = nc.dram_tensor("w_scr", [12, 128, 8, 512], BF16, kind="Internal").ap()

    P = Prog(nc)
    breg = [None]
    greg = [None]

    x_tok = nc.alloc_sbuf_tensor("x_tok", [128, NT, D], F32)
    mod = nc.alloc_sbuf_tensor("mod", [128, 6, D], F32)
    ident_f = nc.alloc_sbuf_tensor("ident_f", [128, 128], F32)
    ident_b = nc.alloc_sbuf_tensor("ident_b", [128, 128], BF16)
    ones_f = nc.alloc_sbuf_tensor("ones_f", [128, 128], F32)
    ones_b = nc.alloc_sbuf_tensor("ones_b", [128, 128], BF16)
    tribias_b = nc.alloc_sbuf_tensor("tribias_b", [128, 128], BF16)
    c_col = nc.alloc_sbuf_tensor("c_col", [128, 8], F32)
    scb_b = nc.alloc_sbuf_tensor("scb_b", [128, 8, 128], BF16)
    stat = nc.alloc_sbuf_tensor("stat", [128, 64], F32)
    ARENA_BYTES = (nc.sbuf_bytes_remaining - 256) // 64 * 64
    arena = nc.alloc_sbuf_tensor("arena", [128, ARENA_BYTES // 4], F32)
    ps = [nc.alloc_psum_tensor("ps%d" % i, [128, 512], F32) for i in range(8)]

    class Arena:
        def __init__(self):
            self.off = 0

        def reset(self):
            self.off = 0

        def get(self, shape, dt):
            n = int(np.prod(shape[1:]))
            esz = 4 if dt == F32 else 2
            nb = (n * esz + 63) // 64 * 64
            off = self.off
            self.off += nb
            assert self.off <= ARENA_BYTES, (self.off, ARENA_BYTES)
            ap = arena[:, off // 4: off // 4 + nb // 4]
            if dt != F32:
                ap = ap.bitcast(dt)
            ap = ap[:, 0:n]
            if len(shape) == 3:
                ap = ap.rearrange("p (a b) -> p a b", a=shape[1])
            elif len(shape) == 4:
                ap = ap.rearrange("p (a b c) -> p a b c", a=shape[1], b=shape[2])
            return ap

    AR = Arena()

    def psb(i):
        return ps[i][:, :].bitcast(BF16)

    def dma(q, out, in_, writes=(), reads=(), key=None):
        return P.op(q, lambda e: e.dma_start(out=out, in_=in_), reads=list(reads), writes=list(writes), dsem=key)

    def pe(fns, reads, writes):
        def run(e):
            r = None
            for f in fns:
                r = f(e)
            return r
        return P.op("pe", run, reads=list(reads), writes=list(writes))

    def mm(out, lhsT, rhs, start, stop):
        return lambda e: e.matmul(out, lhsT=lhsT, rhs=rhs, start=start, stop=stop)

    def tr(out, in_, idn):
        return lambda e: e.transpose(out=out, in_=in_, identity=idn)

    def act(out, in_, func, reads, writes, **kw):
        return P.op("act", lambda e: e.activation(out=out, in_=in_, func=func, **kw), reads=list(reads), writes=list(writes))

    def dve(fn, reads, writes):
        return P.op("dve", fn, reads=list(reads), writes=list(writes))

    XR = lambda t: [("x", t, 0), ("x", t, 1)]

    dma("sp", ident_f[:], ident_d, writes=["ident_f"], key="c_i")
    dma("sp", c_col[:], c_d, writes=["c_col"], key="c_c")
    tb_f = AR.get([128, 128], F32)
    dma("sp", tb_f, tribias_d, writes=["tb_f"], key="c_t")
    dve(lambda e: e.tensor_copy(out=ident_b[:], in_=ident_f[:]), ["ident_f"], ["ident_b"])
    dve(lambda e: e.tensor_copy(out=tribias_b[:], in_=tb_f), ["tb_f"], ["tribias_b"])
    dve(lambda e: e.memset(ones_f[:], 1.0), [], ["ones_f"])
    dve(lambda e: e.memset(ones_b[:], 1.0), [], ["ones_b"])
    sc_col = stat[:, 56:64]
    act(sc_col, c_col[:], AF.Silu, ["c_col"], ["sc_col"])
    dve(lambda e: e.tensor_copy(out=scb_b[:], in_=sc_col.rearrange("p (k o) -> p k o", o=1).to_broadcast([128, 8, 128])),
        ["sc_col"], ["scb"])
    xv = x_d.rearrange("(t p) d -> p t d", p=128)
    for t in range(NT):
        dma("sp", x_tok[:, t, :], xv[:, t, :], writes=XR(t), key=("xl", t % 4))

    def mods(w_ap, b_ap, ncols, spec, base=0):
        P.barrier()
        AR.off = base
        CW = 512
        brow = [AR.get([128, CW], F32) for _ in range(2)]
        nrow = [AR.get([128, CW], F32) for _ in range(2)]
        ntmp = AR.get([128, CW], F32)
        NB_ = 3 if base == 0 else 2
        wbuf = [AR.get([128, 8, CW], BF16) for _ in range(NB_)]
        wv = w_ap.rearrange("(kc p) n -> p kc n", p=128)
        per = D // CW
        for j in range(ncols // CW):
            b = j % 2
            wb_ = j % NB_
            v, q = j // per, j % per
            slot, nr = spec[v]
            dma("sp", brow[b][0:1, :], b_ap[:, j * CW:(j + 1) * CW], writes=[("brow", b)], key=("m_b", b))
            dma("pool", wbuf[wb_], wv[:, :, j * CW:(j + 1) * CW], writes=[("wbuf", wb_)], key=("m_w", wb_))
            fns = [mm(ps[b][:, 0:CW], scb_b[:, kc, :], wbuf[wb_][:, kc, :], kc == 0, False) for kc in range(KC)]
            fns.append(mm(ps[b][:, 0:CW], ones_f[0:1, :], brow[b][0:1, :], False, True))
            pe(fns, [("wbuf", wb_), "scb", ("brow", b), "ones_f"], [("ps", b)])
            dst = mod[:, slot, q * CW:(q + 1) * CW]
            if nr is None:
                act(dst, ps[b][:, 0:CW], AF.Copy, [("ps", b)], [("mod", slot, (q * CW) // 512)])
            else:
                dma("sp", nrow[b][0:1, :], norms_d[nr:nr + 1, q * CW:(q + 1) * CW], writes=[("nrow", b)], key=("m_n", b))
                pe([mm(ps[2 + b][:, 0:CW], ones_f[0:1, :], nrow[b][0:1, :], True, True)],
                   [("nrow", b), "ones_f"], [("ps", 2 + b)])
                act(ntmp, ps[2 + b][:, 0:CW], AF.Copy, [("ps", 2 + b)], ["ntmp"])
                dve(lambda e, dst=dst, b=b: e.scalar_tensor_tensor(out=dst, in0=ps[b][:, 0:CW], scalar=1.0, in1=ntmp,
                                                                      op0=ALU.add, op1=ALU.mult),
                    [("ps", b), "ntmp"], [("mod", slot, (q * CW) // 512)])

    def bcast_row(row_ap, slot):
        P.barrier()
        AR.reset()
        nrow = AR.get([128, D], F32)
        dma("sp", nrow[0:1, :], row_ap, writes=["nrow"], key="m_r")
        for half in range(2):
            pe([mm(ps[half][:, :], ones_f[0:1, :], nrow[0:1, half * 512:(half + 1) * 512], True, True)],
               ["nrow", "ones_f"], [("ps", half)])
            act(mod[:, slot, half * 512:(half + 1) * 512], ps[half][:, :], AF.Copy, [("ps", half)], [("mod", slot, half)])

    MR = lambda s: [("mod", s, 0), ("mod", s, 1)]

    def norm_tile(t, sa, sb, hT, col0, hname, psbank, scr, hbname="hb", hdram=None):
        ss = stat[:, 0:1]
        rs = stat[:, 1:2]
        t1, hb = scr
        dve(lambda e: e.memset(ss, 0.0), [], ["ss"])
        act(hb, x_tok[:, t, :], AF.Square, XR(t) + ["ss"], [hbname, "ss"], accum_out=ss)
        act(rs, ss, AF.Sqrt, ["ss"], ["rs"], bias=1e-6, scale=1.0 / D)
        dve(lambda e: e.reciprocal(out=rs, in_=rs), ["rs"], ["rs"])
        dve(lambda e: e.scalar_tensor_tensor(out=t1, in0=x_tok[:, t, :], scalar=rs, in1=mod[:, sa, :], op0=ALU.mult, op1=ALU.mult),
            XR(t) + ["rs"] + MR(sa), ["t1"])
        dve(lambda e: e.tensor_tensor(out=hb, in0=t1, in1=mod[:, sb, :], op=ALU.add), ["t1"] + MR(sb), [hbname])
        if hdram is not None:
            dma("sp", hdram, hb, reads=[hbname], writes=[("hdram", t)], key=("hd", t % 2))
        pb = psb(psbank)
        pe([tr(pb[:, kc * 128:(kc + 1) * 128], hb[:, kc * 128:(kc + 1) * 128], ident_b[:]) for kc in range(KC)],
           [hbname, "ident_b"], [("ps", psbank)])
        act(hT[:, :, col0:col0 + 128], pb.rearrange("p (k c) -> p k c", k=KC), AF.Copy, [("ps", psbank)], [hname])

    def wload(dst, src, wname, key):
        dma("pool", dst, src, writes=[wname], key=key)

    def wload2(dst, src, wname, key):
        for hf in range(2):
            dma("pool", dst[:, :, hf * 512:(hf + 1) * 512], src[:, :, hf * 512:(hf + 1) * 512], writes=[(wname, hf)], key=(key, hf))

    def gmlp_phase():
        P.barrier()
        AR.reset()
        TG = 256
        NG = S // TG
        hT_G = AR.get([128, KC, TG], BF16)
        NCH = 4
        wch = [AR.get([128, 8, 512], BF16) for _ in range(NCH)]
        uT_G = AR.get([128, 16, TG], BF16)
        yT_G = AR.get([128, 16, TG], BF16)
        vf = AR.get([128, 2, 2048], F32)
        vn = AR.get([128, 2, 2048], BF16)
        lnG = AR.get([128, 2048], F32)
        lnB = AR.get([128, 2048], F32)
        WmT = AR.get([128, 8, 128], BF16)
        t1 = AR.get([128, D], F32)
        hb = AR.get([128, D], BF16)
        rows = vf[:, 0, :]
        biv_hi = AR.get([128, 2048], BF16)
        biv_lo = AR.get([128, 2048], BF16)
        bs_hi = AR.get([128, 1024], BF16)
        bs_lo = AR.get([128, 1024], BF16)
        biu = AR.get([128, 16], F32)
        tmpf = t1
        for which, dst in ((0, lnG), (1, lnB)):
            dma("sp", rows[0:1, :], a_ln_d[which:which + 1, :], writes=["rows"], key="g_r")
            for j in range(4):
                pe([mm(ps[j][:, :], ones_f[0:1, :], rows[0:1, j * 512:(j + 1) * 512], True, True)], ["rows", "ones_f"], [("ps", j)])
                act(dst[:, j * 512:(j + 1) * 512], ps[j][:, :], AF.Copy, [("ps", j)], [("ln", which)])
        dma("sp", rows[0:1, :], a_b_in_v_d, writes=["rows"], key="g_r")
        dve(lambda e: e.tensor_copy(out=biv_hi[0:1, :], in_=rows[0:1, :]), ["rows"], ["biv_hi"])
        dve(lambda e: e.tensor_tensor(out=rows[0:1, :], in0=rows[0:1, :], in1=biv_hi[0:1, :], op=ALU.subtract), ["rows", "biv_hi"], ["rows"])
        dve(lambda e: e.tensor_copy(out=biv_lo[0:1, :], in_=rows[0:1, :]), ["rows"], ["biv_lo"])
        dma("sp", rows[0:1, 0:1024], a_b_s_d, writes=["rows"], key="g_r")
        dve(lambda e: e.tensor_copy(out=bs_hi[0:1, :], in_=rows[0:1, 0:1024]), ["rows"], ["bs_hi"])
        dve(lambda e: e.tensor_tensor(out=rows[0:1, 0:1024], in0=rows[0:1, 0:1024], in1=bs_hi[0:1, :], op=ALU.subtract), ["rows", "bs_hi"], ["rows"])
        dve(lambda e: e.tensor_copy(out=bs_lo[0:1, :], in_=rows[0:1, 0:1024]), ["rows"], ["bs_lo"])
        dma("sp", biu, a_b_in_u_d, writes=["biu"], key="g_biu")
        wsf = tmpf.rearrange("p (g t) -> p g t", g=8)
        dma("sp", wsf, a_w_sT_d, writes=["t1"], key="g_ws")
        tmk = hb.bitcast(F32)[:, 0:128]
        dma("sp", tmk, trimask_d, writes=["tmk"], key="g_tm")
        dve(lambda e: e.tensor_tensor(out=WmT, in0=wsf, in1=tmk.rearrange("p (o t) -> p o t", o=1).to_broadcast([128, 8, 128]), op=ALU.mult),
            ["t1", "tmk"], ["WmT"])
        P.barrier()

        w_in_v = a_w_in_d.rearrange("(kc p) n -> p kc n", p=128)
        w_out_v = a_w_out_d.rearrange("(fc p) n -> p fc n", p=128)
        nch = [0]

        def next_chunk(src):
            b = nch[0] % NCH
            cid = nch[0] % 12
            first = nch[0] < 12
            nch[0] += 1
            if first:
                wload(wch[b], src, ("wch", b), ("g_w", b))
                dma("sp", wscr[cid], wch[b], reads=[("wch", b)], writes=[("wscr", cid)], key=("g_ws2", cid % 4))
            else:
                dma("sp", wch[b], wscr[cid], reads=[("wscr", cid)], writes=[("wch", b)], key=("g_w2", b))
            return b

        for G in range(NG):
            for tt in range(2):
                norm_tile(2 * G + tt, 0, 1, hT_G, tt * 128, "hT_G", 7, (t1, hb))
            for c in range(4):
                b = next_chunk(w_in_v[:, :, c * 512:(c + 1) * 512])
                for f in range(4):
                    fc = c * 4 + f
                    pb = fc % 2
                    pe([mm(ps[pb][:, 0:TG], wch[b][:, kc, f * 128:(f + 1) * 128], hT_G[:, kc, :], kc == 0, kc == KC - 1) for kc in range(KC)],
                       [("wch", b), "hT_G"], [("ps", pb)])
                    act(uT_G[:, fc, :], ps[pb][:, 0:TG], AF.Gelu, [("ps", pb), "biu"], [("uT", fc)], bias=biu[:, fc:fc + 1], scale=1.0)
            dve(lambda e: e.memset(stat[:, 8:16], 0.0), [], [("vs", tt, c) for tt in range(2) for c in range(4)])
            for c in range(4):
                b = next_chunk(w_in_v[:, :, 2048 + c * 512: 2048 + (c + 1) * 512])
                for tt in range(2):
                    pb = 2 + (c * 2 + tt) % 2
                    fns = [mm(ps[pb][:, :], hT_G[:, kc, tt * 128:(tt + 1) * 128], wch[b][:, kc, :], kc == 0, False) for kc in range(KC)]
                    fns.append(mm(ps[pb][:, :], ones_b[0:1, :], biv_hi[0:1, c * 512:(c + 1) * 512], False, False))
                    fns.append(mm(ps[pb][:, :], ones_b[0:1, :], biv_lo[0:1, c * 512:(c + 1) * 512], False, True))
                    pe(fns, [("wch", b), "hT_G", "ones_b", "biv_hi", "biv_lo"], [("ps", pb)])
                    act(vf[:, tt, c * 512:(c + 1) * 512], ps[pb][:, :], AF.Gelu, [("ps", pb)], [("vf", tt, c), ("vs", tt, c)],
                        accum_out=stat[:, 8 + tt * 4 + c: 9 + tt * 4 + c])
            def sc_(tt, k):
                return stat[:, 16 + tt * 8 + k: 17 + tt * 8 + k]
            TT = range(2)
            vts = [vf[:, tt, :] for tt in TT]
            vfrs = [[("vf", tt, c) for c in range(4)] for tt in TT]
            for tt in TT:
                s1, s2 = sc_(tt, 0), sc_(tt, 1)
                dve(lambda e, tt=tt, s1=s1: e.reduce_sum(out=s1, in_=stat[:, 8 + tt * 4: 12 + tt * 4], axis=AX.X), [("vs", tt, c) for c in range(4)], [("s1", tt)])
                dve(lambda e, s2=s2: e.memset(s2, 0.0), [], [("s2", tt)])
                act(vn[:, tt, :], vts[tt], AF.Square, vfrs[tt] + [("s2", tt)], [("vn", tt), ("s2", tt)], accum_out=s2)
            for tt in TT:
                s1, s2, mu, var = sc_(tt, 0), sc_(tt, 1), sc_(tt, 2), sc_(tt, 3)
                dve(lambda e, mu=mu, s1=s1: e.tensor_scalar(out=mu, in0=s1, scalar1=1.0 / 2048, scalar2=None, op0=ALU.mult), [("s1", tt)], [("mu", tt)])
                dve(lambda e, mu=mu, var=var: e.tensor_tensor(out=var, in0=mu, in1=mu, op=ALU.mult), [("mu", tt)], [("var", tt)])
                dve(lambda e, s2=s2, var=var: e.scalar_tensor_tensor(out=var, in0=s2, scalar=1.0 / 2048, in1=var, op0=ALU.mult, op1=ALU.subtract),
                    [("s2", tt), ("var", tt)], [("var", tt)])
            for tt in TT:
                act(sc_(tt, 4), sc_(tt, 3), AF.Sqrt, [("var", tt)], [("rstd", tt)], bias=1e-5, scale=1.0)
            for tt in TT:
                mu, rstd, nmr = sc_(tt, 2), sc_(tt, 4), sc_(tt, 5)
                dve(lambda e, rstd=rstd: e.reciprocal(out=rstd, in_=rstd), [("rstd", tt)], [("rstd", tt)])
                dve(lambda e, mu=mu, rstd=rstd, nmr=nmr: e.scalar_tensor_tensor(out=nmr, in0=mu, scalar=-1.0, in1=rstd, op0=ALU.mult, op1=ALU.mult),
                    [("mu", tt), ("rstd", tt)], [("nmr", tt)])
            for tt in TT:
                act(vts[tt], vts[tt], AF.Identity, vfrs[tt] + [("rstd", tt), ("nmr", tt)], vfrs[tt], bias=sc_(tt, 5), scale=sc_(tt, 4))
            for tt in TT:
                dve(lambda e, vt=vts[tt]: e.tensor_tensor(out=vt, in0=vt, in1=lnG, op=ALU.mult), vfrs[tt] + [("ln", 0)], vfrs[tt])
                dve(lambda e, vt=vts[tt], tt=tt: e.tensor_tensor(out=vn[:, tt, :], in0=vt, in1=lnB, op=ALU.add), vfrs[tt] + [("ln", 1)], [("vn", tt)])
            for tt in TT:
                for q4 in range(4):
                    bank = 4 + q4 if q4 < 3 else 0
                    fns = []
                    for f in range(4):
                        fc = q4 * 4 + f
                        g = fc // 2
                        o = ps[bank][:, f * 128:(f + 1) * 128]
                        fns.append(mm(o, vn[:, tt, fc * 128:(fc + 1) * 128], WmT[:, g, :], True, False))
                        fns.append(mm(o, ones_b[0:1, :], bs_hi[0:1, g * 128:(g + 1) * 128], False, False))
                        fns.append(mm(o, ones_b[0:1, :], bs_lo[0:1, g * 128:(g + 1) * 128], False, True))
                    pe(fns, [("vn", tt), "WmT", "ones_b", "bs_hi", "bs_lo"], [("ps", bank)])
                    dve(lambda e, bank=bank, q4=q4, tt=tt: e.tensor_tensor(
                        out=yT_G[:, q4 * 4:(q4 + 1) * 4, tt * 128:(tt + 1) * 128],
                        in0=ps[bank][:, :].rearrange("p (f t) -> p f t", f=4),
                        in1=uT_G[:, q4 * 4:(q4 + 1) * 4, tt * 128:(tt + 1) * 128], op=ALU.mult),
                        [("ps", bank)] + [("uT", q4 * 4 + f) for f in range(4)], [("yT", tt, q4)])
            for dh in range(2):
                for fh in range(2):
                    b = next_chunk(w_out_v[:, fh * 8:(fh + 1) * 8, dh * 512:(dh + 1) * 512])
                    for tt in range(2):
                        bank = 1 + tt
                        pe([mm(ps[bank][:, :], yT_G[:, fh * 8 + f, tt * 128:(tt + 1) * 128], wch[b][:, f, :], fh == 0 and f == 0, fh == 1 and f == 7)
                            for f in range(8)],
                           [("wch", b)] + [("yT", tt, q4) for q4 in range(4)], [("ps", bank)])
                for tt in range(2):
                    bank = 1 + tt
                    t = 2 * G + tt
                    tm = tmpf[:, 0:512]
                    dve(lambda e, bank=bank, dh=dh: e.tensor_tensor(out=tm, in0=ps[bank][:, :], in1=mod[:, 2, dh * 512:(dh + 1) * 512], op=ALU.mult),
                        [("ps", bank), ("mod", 2, dh)], ["t1"])
                    dve(lambda e, t=t, dh=dh: e.tensor_tensor(out=x_tok[:, t, dh * 512:(dh + 1) * 512], in0=x_tok[:, t, dh * 512:(dh + 1) * 512], in1=tm, op=ALU.add),
                        ["t1", ("x", t, dh)], [("x", t, dh)])

    def moe_phase(li):
        P.barrier()
        AR.reset()
        hT = AR.get([128, KC, S], BF16)
        NWB = 3
        wgb = [AR.get([128, 8, 256], BF16) for _ in range(NWB)]
        wub = [AR.get([128, 8, 256], BF16) for _ in range(NWB)]
        wdb = [AR.get([128, 2, D], BF16) for _ in range(NWB)]
        hid = [AR.get([128, 2, 512], BF16) for _ in range(2)]
        sg = [AR.get([128, 512], F32) for _ in range(2)]
        wr = AR.get([128, NT, 64], F32)
        wrb = AR.get([128, 8, 64], BF16)
        mbb = AR.get([128, 64], F32)
        t1 = AR.get([128, D], F32)
        hb = AR.get([128, D], BF16)
        rt = AR.get([128, 8, 64], F32)
        wload(wrb, router_d[li].rearrange("(kc p) n -> p kc n", p=128), "wrb", "r_w")
        dma("sp", t1[0:1, 0:64], mbias_d[li:li + 1, :], writes=["mrow"], key="r_b")
        pe([mm(ps[0][:, 0:64], ones_f[0:1, :], t1[0:1, 0:64], True, True)], ["mrow", "ones_f"], [("ps", 0)])
        act(mbb, ps[0][:, 0:64], AF.Copy, [("ps", 0)], ["mbb"])
        P.barrier()
        for t in range(NT):
            norm_tile(t, 3, 4, hT, t * 128, ("hT", t // 4), t % 2, (t1, hb))
        sc = rt[:, 0, :]; ch = rt[:, 1, :]; eq = rt[:, 2, :]; c2 = rt[:, 3, :]; cm = rt[:, 4, :]; wsel = rt[:, 5, :]
        m1 = rt[:, 6, 0:8]; m2 = rt[:, 6, 8:16]; gs = rt[:, 6, 16:24]; g8 = rt[:, 6, 24:32]; gm = rt[:, 6, 32:40]; e8 = rt[:, 6, 40:48]
        wsum = rt[:, 6, 48:49]
        g3 = lambda a: a.rearrange("p (g k) -> p g k", k=8)
        b3 = lambda a: a.rearrange("p (g o) -> p g o", o=1).to_broadcast([128, 8, 8])
        for t in range(NT):
            bank = 2 + t % 2
            pe([mm(ps[bank][:, 0:64], hT[:, kc, t * 128:(t + 1) * 128], wrb[:, kc, :], kc == 0, kc == KC - 1) for kc in range(KC)],
               [("hT", t // 4), "wrb"], [("ps", bank)])
            act(sc, ps[bank][:, 0:64], AF.Sigmoid, [("ps", bank)], ["sc"])
            dve(lambda e: e.tensor_tensor(out=ch, in0=sc, in1=mbb, op=ALU.add), ["sc", "mbb"], ["ch"])
            dve(lambda e: e.tensor_reduce(out=m1, in_=g3(ch), axis=AX.X, op=ALU.max), ["ch"], ["m1"])
            dve(lambda e: e.tensor_tensor(out=g3(eq), in0=g3(ch), in1=b3(m1), op=ALU.is_ge), ["ch", "m1"], ["eq"])
            dve(lambda e: e.scalar_tensor_tensor(out=c2, in0=eq, scalar=-1e30, in1=ch, op0=ALU.mult, op1=ALU.add), ["eq", "ch"], ["c2"])
            dve(lambda e: e.tensor_reduce(out=m2, in_=g3(c2), axis=AX.X, op=ALU.max), ["c2"], ["m2"])
            dve(lambda e: e.tensor_tensor(out=gs, in0=m1, in1=m2, op=ALU.add), ["m1", "m2"], ["gs"])
            dve(lambda e: e.max(out=g8, in_=gs), ["gs"], ["g8"])
            dve(lambda e: e.tensor_scalar(out=gm, in0=gs, scalar1=g8[:, 3:4], scalar2=None, op0=ALU.is_ge), ["gs", "g8"], ["gm"])
            dve(lambda e: e.scalar_tensor_tensor(out=g3(cm), in0=g3(ch), scalar=2.0, in1=b3(gm), op0=ALU.add, op1=ALU.mult), ["ch", "gm"], ["cm"])
            dve(lambda e: e.max(out=e8, in_=cm), ["cm"], ["e8"])
            dve(lambda e: e.scalar_tensor_tensor(out=wsel, in0=cm, scalar=e8[:, 7:8], in1=sc, op0=ALU.is_ge, op1=ALU.mult), ["cm", "e8", "sc"], ["wsel"])
            dve(lambda e: e.reduce_sum(out=wsum, in_=wsel, axis=AX.X), ["wsel"], ["wsum"])
            dve(lambda e: e.reciprocal(out=wsum, in_=wsum), ["wsum"], ["wsum"])
            dve(lambda e, t=t: e.tensor_scalar(out=wr[:, t, :], in0=wsel, scalar1=wsum, scalar2=2.5, op0=ALU.mult, op1=ALU.mult),
                ["wsel", "wsum"], [("wr", t)])
        G2 = 5
        for ei in range(65):
            b = ei % NWB
            if ei < 64:
                gsrc, usrc, dsrc = wg_d[li, ei], wu_d[li, ei], wd_d[li, ei]
            else:
                gsrc, usrc, dsrc = swg_d[li], swu_d[li], swd_d[li]
            wload(wgb[b], gsrc.rearrange("(kc p) n -> p kc n", p=128), ("wg", b), ("e_wg", b))
            wload(wub[b], usrc.rearrange("(kc p) n -> p kc n", p=128), ("wu", b), ("e_wu", b))
            wload(wdb[b], dsrc.rearrange("(fc p) n -> p fc n", p=128), ("wd", b), ("e_wd", b))
            dve(lambda e, b=b: e.tensor_tensor(out=wdb[b], in0=wdb[b], in1=mod[:, G2:G2 + 1, :].to_broadcast([128, 2, D]), op=ALU.mult),
                [("wd", b)] + MR(G2), [("wd", b)])
            for tg in range(4):
                hb_i = (ei * 4 + tg) % 2
                for f in range(2):
                    pe([mm(ps[f][:, :], wgb[b][:, kc, f * 128:(f + 1) * 128], hT[:, kc, tg * 512:(tg + 1) * 512], kc == 0, kc == KC - 1) for kc in range(KC)],
                       [("wg", b), ("hT", tg)], [("ps", f)])
                    pe([mm(ps[2 + f][:, :], wub[b][:, kc, f * 128:(f + 1) * 128], hT[:, kc, tg * 512:(tg + 1) * 512], kc == 0, kc == KC - 1) for kc in range(KC)],
                       [("wu", b), ("hT", tg)], [("ps", 2 + f)])
                    act(sg[f], ps[f][:, :], AF.Silu, [("ps", f)], [("sg", f)])
                    dve(lambda e, f=f, hb_i=hb_i: e.tensor_tensor(out=hid[hb_i][:, f, :], in0=sg[f], in1=ps[2 + f][:, :], op=ALU.mult),
                        [("sg", f), ("ps", 2 + f)], [("hid", hb_i, f)])
                for tt in range(4):
                    t = tg * 4 + tt
                    for dh in range(2):
                        bank = 4 + (tt * 2 + dh) % 4
                        pe([mm(ps[bank][:, :], hid[hb_i][:, f, tt * 128:(tt + 1) * 128], wdb[b][:, f, dh * 512:(dh + 1) * 512], f == 0, f == 1) for f in range(2)],
                           [("hid", hb_i, 0), ("hid", hb_i, 1), ("wd", b)], [("ps", bank)])
                        xs = x_tok[:, t, dh * 512:(dh + 1) * 512]
                        scal = wr[:, t, ei:ei + 1] if ei < 64 else 1.0
                        dve(lambda e, bank=bank, xs=xs, scal=scal: e.scalar_tensor_tensor(out=xs, in0=ps[bank][:, :], scalar=scal, in1=xs, op0=ALU.mult, op1=ALU.add),
                            [("ps", bank), ("x", t, dh), ("wr", t)], [("x", t, dh)])


    def moe_sparse(li):
        P.barrier()
        AR.reset()
        C = CAP
        NS = C // 128
        G2 = 5
        wr = AR.get([128, NT, 64], F32)
        posm = AR.get([128, NT, 64], F32)
        Rall = AR.get([128, NT * 64, 6], BF16)
        iotaC = AR.get([128, C], F32)
        persist = AR.off
        hT = AR.get([128, KC, S], BF16)
        mask_b = AR.get([128, NT, 64], BF16)
        wgb0 = AR.get([128, 8, 256], BF16)
        wub0 = AR.get([128, 8, 256], BF16)
        wdb0 = AR.get([128, 2, D], BF16)
        hid = [AR.get([128, 2, 512], BF16) for _ in range(2)]
        sg = [AR.get([128, 512], F32) for _ in range(2)]
        wrb = AR.get([128, 8, 64], BF16)
        mbb = AR.get([128, 64], F32)
        t1 = AR.get([128, D], F32)
        hbs = [AR.get([128, D], BF16) for _ in range(2)]
        R_sc = AR.get([128, NT * 64], F32)
        R_ch = AR.get([128, NT * 64], F32)
        R_t = AR.get([128, NT * 64], F32)
        tix = AR.get([128, NT * 64], F32)
        rm1 = AR.get([128, 128], F32)
        rm2 = AR.get([128, 128], F32)
        rgs = AR.get([128, 128], F32)
        rgm = AR.get([128, 128], F32)
        rg8 = AR.get([128, NT, 8], F32)
        re8 = AR.get([128, NT, 8], F32)
        rws = AR.get([128, NT], F32)
        U_b = AR.get([128, 64], BF16)
        L_b = AR.get([128, 128], BF16)
        maskT_all = AR.get([128, NT * 128], BF16)
        pidx = AR.get([128, 1], F32)
        dma("sp", iotaC, iota_d, writes=["iotaC"], key="k_io")
        dma("sp", pidx, pidx_d, writes=["pidx"], key="k_pi")
        dma("sp", tix, tix_d, writes=["tix"], key="k_ti")
        dma("pool", U_b[0:64, :], ustrict_d, writes=["U_b"], key="k_u")
        dma("pool", L_b, lstrict_d, writes=["L_b"], key="k_l")
        wload(wrb, router_d[li].rearrange("(kc p) n -> p kc n", p=128), "wrb", "r_w")
        dma("sp", t1[0:1, 0:64], mbias_d[li:li + 1, :], writes=["mrow"], key="r_b")
        pe([mm(ps[0][:, 0:64], ones_f[0:1, :], t1[0:1, 0:64], True, True)], ["mrow", "ones_f"], [("ps", 0)])
        act(mbb, ps[0][:, 0:64], AF.Copy, [("ps", 0)], ["mbb"])
        wload(wgb0, swg_d[li].rearrange("(kc p) n -> p kc n", p=128), "wg0", "e_wg0")
        wload(wub0, swu_d[li].rearrange("(kc p) n -> p kc n", p=128), "wu0", "e_wu0")
        wload(wdb0, swd_d[li].rearrange("(fc p) n -> p fc n", p=128), "wd0", "e_wd0")
        P.barrier()
        hv = h_dram.rearrange("(t p) d -> t p d", p=128)
        for t in range(NT):
            norm_tile(t, 3, 4, hT, t * 128, ("hT", t // 4), t % 2, (t1, hbs[t % 2]), hbname=("hb", t % 2), hdram=hv[t])
        v3 = lambda a_: a_.rearrange("p (g k) -> p g k", k=8)
        t3 = lambda a_: a_.rearrange("p (t e) -> p t e", e=64)
        bc = lambda a_, n: a_.rearrange("p (g o) -> p g o", o=1).to_broadcast([128, a_.shape[1], n])
        for t in range(NT):
            bank = 2 + t // 8
            pe([mm(ps[bank][:, (t % 8) * 64:(t % 8 + 1) * 64], hT[:, kc, t * 128:(t + 1) * 128], wrb[:, kc, :], kc == 0, kc == KC - 1) for kc in range(KC)],
               [("hT", t // 4), "wrb"], [("ps", bank)])
        for j in range(2):
            act(R_sc[:, j * 512:(j + 1) * 512], ps[2 + j][:, :], AF.Sigmoid, [("ps", 2 + j)], ["R_sc"])
        dve(lambda e: e.tensor_tensor(out=t3(R_ch), in0=t3(R_sc), in1=mbb.rearrange("p (o e) -> p o e", o=1).to_broadcast([128, NT, 64]), op=ALU.add),
            ["R_sc", "mbb"], ["R_ch"])
        dve(lambda e: e.tensor_reduce(out=rm1, in_=v3(R_ch), axis=AX.X, op=ALU.max), ["R_ch"], ["rm1"])
        dve(lambda e: e.tensor_tensor(out=v3(R_t), in0=v3(R_ch), in1=bc(rm1, 8), op=ALU.is_ge), ["R_ch", "rm1"], ["R_t"])
        dve(lambda e: e.scalar_tensor_tensor(out=R_t, in0=R_t, scalar=-1e30, in1=R_ch, op0=ALU.mult, op1=ALU.add), ["R_t", "R_ch"], ["R_t"])
        dve(lambda e: e.tensor_reduce(out=rm2, in_=v3(R_t), axis=AX.X, op=ALU.max), ["R_t"], ["rm2"])
        dve(lambda e: e.tensor_tensor(out=rgs, in0=rm1, in1=rm2, op=ALU.add), ["rm1", "rm2"], ["rgs"])
        rgs3 = rgs.rearrange("p (t g) -> p t g", g=8)
        for t in range(NT):
            dve(lambda e, t=t: e.max(out=rg8[:, t, :], in_=rgs3[:, t, :]), ["rgs"], ["rg8"])
        dve(lambda e: e.tensor_tensor(out=rgm.rearrange("p (t g) -> p t g", g=8), in0=rgs3, in1=rg8[:, :, 3:4].to_broadcast([128, NT, 8]), op=ALU.is_ge),
            ["rgs", "rg8"], ["rgm"])
        dve(lambda e: e.scalar_tensor_tensor(out=v3(R_t), in0=v3(R_ch), scalar=2.0, in1=bc(rgm, 8), op0=ALU.add, op1=ALU.mult), ["R_ch", "rgm"], ["R_t"])
        for t in range(NT):
            dve(lambda e, t=t: e.max(out=re8[:, t, :], in_=t3(R_t)[:, t, :]), ["R_t"], ["re8"])
        dve(lambda e: e.tensor_tensor(out=t3(R_ch), in0=t3(R_t), in1=re8[:, :, 7:8].to_broadcast([128, NT, 64]), op=ALU.is_ge), ["R_t", "re8"], ["R_ch"])
        dve(lambda e: e.tensor_copy(out=mask_b.rearrange("p t e -> p (t e)"), in_=R_ch), ["R_ch"], [("mask", t) for t in range(NT)])
        dve(lambda e: e.tensor_tensor(out=R_t, in0=R_ch, in1=R_sc, op=ALU.mult), ["R_ch", "R_sc"], ["R_t"])
        dve(lambda e: e.tensor_reduce(out=rws, in_=t3(R_t), axis=AX.X, op=ALU.add), ["R_t"], ["rws"])
        dve(lambda e: e.reciprocal(out=rws, in_=rws), ["rws"], ["rws"])
        dve(lambda e: e.scalar_tensor_tensor(out=wr, in0=t3(R_t), scalar=2.5, in1=bc(rws, 64), op0=ALU.mult, op1=ALU.mult),
            ["R_t", "rws"], [("wr", t) for t in range(NT)])
        dve(lambda e: e.tensor_tensor(out=wdb0, in0=wdb0, in1=mod[:, G2:G2 + 1, :].to_broadcast([128, 2, D]), op=ALU.mult), ["wd0"] + MR(G2), ["wd0"])
        for tg in range(4):
            hb_i = tg % 2
            for f in range(2):
                pe([mm(ps[f][:, :], wgb0[:, kc, f * 128:(f + 1) * 128], hT[:, kc, tg * 512:(tg + 1) * 512], kc == 0, kc == KC - 1) for kc in range(KC)],
                   ["wg0", ("hT", tg)], [("ps", f)])
                pe([mm(ps[2 + f][:, :], wub0[:, kc, f * 128:(f + 1) * 128], hT[:, kc, tg * 512:(tg + 1) * 512], kc == 0, kc == KC - 1) for kc in range(KC)],
                   ["wu0", ("hT", tg)], [("ps", 2 + f)])
                act(sg[f], ps[f][:, :], AF.Silu, [("ps", f)], [("sg", f)])
                dve(lambda e, f=f, hb_i=hb_i: e.tensor_tensor(out=hid[hb_i][:, f, :], in0=sg[f], in1=ps[2 + f][:, :], op=ALU.mult),
                    [("sg", f), ("ps", 2 + f)], [("hid", hb_i, f)])
            for tt in range(4):
                t = tg * 4 + tt
                for dh in range(2):
                    bank = 4 + (tt * 2 + dh) % 4
                    pe([mm(ps[bank][:, :], hid[hb_i][:, f, tt * 128:(tt + 1) * 128], wdb0[:, f, dh * 512:(dh + 1) * 512], f == 0, f == 1) for f in range(2)],
                       [("hid", hb_i, 0), ("hid", hb_i, 1), "wd0"], [("ps", bank)])
                    xs = x_tok[:, t, dh * 512:(dh + 1) * 512]
                    dve(lambda e, bank=bank, xs=xs: e.tensor_tensor(out=xs, in0=ps[bank][:, :], in1=xs, op=ALU.add),
                        [("ps", bank), ("x", t, dh)], [("x", t, dh)])
        mrd = [("mask", t) for t in range(NT)]
        for i in range(NT):
            bank = 4 + i // 8
            o = ps[bank][:, (i % 8) * 64:(i % 8 + 1) * 64]
            fns = [mm(o, ones_b[:], mask_b[:, j, :], j == 0, False) for j in range(i)]
            fns.append(mm(o, L_b, mask_b[:, i, :], i == 0, True))
            pe(fns, mrd + ["ones_b", "L_b"], [("ps", bank)])
        for j in range(2):
            pm = posm.rearrange("p t e -> p (t e)")[:, j * 512:(j + 1) * 512]
            dve(lambda e, pm=pm, j=j: e.scalar_tensor_tensor(out=pm, in0=ps[4 + j][:, :], scalar=1.0, in1=R_ch[:, j * 512:(j + 1) * 512], op0=ALU.add, op1=ALU.mult),
                [("ps", 4 + j), "R_ch"], [("posm", t) for t in range(NT)])
            dve(lambda e, pm=pm: e.tensor_scalar(out=pm, in0=pm, scalar1=-1.0, scalar2=None, op0=ALU.add),
                [("posm", t) for t in range(NT)], [("posm", t) for t in range(NT)])
        for j in range(2):
            pbk = psb(6 + j)
            pe([tr(pbk[0:64, (i % 8) * 128:(i % 8 + 1) * 128], mask_b[:, i, :], ident_b[:]) for i in range(j * 8, j * 8 + 8)],
               mrd + ["ident_b"], [("ps", 6 + j)])
            act(maskT_all[0:64, j * 1024:(j + 1) * 1024], pbk[0:64, :], AF.Copy, [("ps", 6 + j)], [("maskT_all", j)])
        for i in range(NT):
            bank = 2 + i // 8
            pe([mm(ps[bank][:, (i % 8) * 64:(i % 8 + 1) * 64], maskT_all[0:64, i * 128:(i + 1) * 128], U_b[0:64, :], True, True)],
               [("maskT_all", i // 8), "U_b"], [("ps", bank)])
        RR = [("R", i, c) for i in range(NT) for c in range(6)]
        c1 = lambda a_: a_.rearrange("p (a o) -> p a o", o=1)
        dve(lambda e: e.tensor_copy(out=Rall[:, :, 0:1], in_=c1(tix)), ["tix"], RR)
        dve(lambda e: e.tensor_copy(out=Rall[:, :, 1:2], in_=pidx.rearrange("p (a o) -> p a o", o=1).to_broadcast([128, NT * 64, 1])), ["pidx"], RR)
        dve(lambda e: e.memset(Rall[:, :, 2:3], 1.0), [], RR)
        for j in range(2):
            dve(lambda e, j=j: e.tensor_copy(out=Rall[:, j * 512:(j + 1) * 512, 3:4], in_=c1(ps[2 + j][:, :])), [("ps", 2 + j)], RR)
        wrf = wr.rearrange("p t e -> p (t e)")
        dve(lambda e: e.tensor_copy(out=Rall[:, :, 4:5], in_=c1(wrf)), [("wr", t) for t in range(NT)], RR)
        dve(lambda e: e.tensor_tensor(out=Rall[:, :, 5:6], in0=c1(wrf), in1=Rall[:, :, 4:5], op=ALU.subtract), [("wr", t) for t in range(NT)] + RR, RR)
        P.barrier()
        AR.off = persist
        NWB = 2
        wgb = [AR.get([128, 8, 256], BF16) for _ in range(NWB)]
        wub = [AR.get([128, 8, 256], BF16) for _ in range(NWB)]
        wdb = [AR.get([128, 2, D], BF16) for _ in range(NWB)]
        hg = [AR.get([128, NS, D], BF16) for _ in range(2)]
        hgT = [AR.get([128, KC, C], BF16) for _ in range(2)]
        hid2 = [AR.get([128, 2, C], BF16) for _ in range(2)]
        sg2 = [AR.get([128, C], BF16) for _ in range(2)]
        yout = AR.get([128, NS, D], F32)
        OH = [AR.get([128, C], BF16) for _ in range(8)]
        sl = [AR.get([128, NS, 10], F32) for _ in range(4)]
        sli = [AR.get([128, NS, 2], I32) for _ in range(4)]
        wsl = [AR.get([128, NS], F32) for _ in range(4)]
        NE = int(os.environ.get("NEXP", "64"))
        for hb_ in range(2):
            dve(lambda e, hb_=hb_: e.memset(hg[hb_].rearrange("p a b -> p (a b)"), 0.0), [], [("hg", hb_, s_) for s_ in range(NS)])

        def weights(ei):
            wb = ei % NWB
            wload(wgb[wb], wg_d[li, ei].rearrange("(kc p) n -> p kc n", p=128), ("wg", wb), ("e_wg", wb))
            wload(wub[wb], wu_d[li, ei].rearrange("(kc p) n -> p kc n", p=128), ("wu", wb), ("e_wu", wb))
            wload(wdb[wb], wd_d[li, ei].rearrange("(fc p) n -> p fc n", p=128), ("wd", wb), ("e_wd", wb))

        def g2fold(ei):
            wb = ei % NWB
            dve(lambda e, wb=wb: e.tensor_tensor(out=wdb[wb], in0=wdb[wb], in1=mod[:, G2:G2 + 1, :].to_broadcast([128, 2, D]), op=ALU.mult),
                [("wd", wb)] + MR(G2), [("wd", wb)])

        def ohs(ei, half):
            for i in range(half * 8, half * 8 + 8):
                o = i % 8
                dve(lambda e, o=o, i=i, ei=ei: e.tensor_scalar(out=OH[o], in0=iotaC, scalar1=posm[:, i, ei:ei + 1], scalar2=None, op0=ALU.is_equal),
                    ["iotaC", ("posm", i)], [("OH", o)])

        def pe_idx(ei, half):
            ib = 7
            fns = []
            for i in range(half * 8, half * 8 + 8):
                o = i % 8
                for s_ in range(NS):
                    fns.append(mm(ps[ib][:, s_ * 8:s_ * 8 + 6], OH[o][:, s_ * 128:(s_ + 1) * 128], Rall[:, i * 64 + ei, :],
                                  i == 0 and s_ == 0, i == NT - 1 and s_ == NS - 1))
            pe(fns, [("OH", o) for o in range(8)] + [("R", i, c) for i in range(half * 8, half * 8 + 8) for c in range(6)], [("ps", ib)])

        def slot_math(ei):
            b3_ = ei % 4
            ib = 7
            pv = ps[ib][:, 0:NS * 8].rearrange("p (s c) -> p s c", c=8)
            S_ = sl[b3_]
            rg = [("sl", b3_)]
            dve(lambda e: e.tensor_copy(out=S_[:, :, 0:6], in_=pv[:, :, 0:6]), [("ps", ib)], rg)
            dve(lambda e: e.scalar_tensor_tensor(out=S_[:, :, 6:7], in0=S_[:, :, 0:1], scalar=128.0, in1=S_[:, :, 1:2], op0=ALU.mult, op1=ALU.add), rg, rg)
            dve(lambda e: e.scalar_tensor_tensor(out=S_[:, :, 8:9], in0=S_[:, :, 6:7], scalar=-1.0, in1=S_[:, :, 2:3], op0=ALU.add, op1=ALU.add), rg, rg)
            dve(lambda e: e.tensor_copy(out=sli[b3_][:, :, 0:1], in_=S_[:, :, 8:9]), rg, [("slig", b3_)])

        def slot_math2(ei):
            b3_ = ei % 4
            S_ = sl[b3_]
            rg = [("sl", b3_)]
            dve(lambda e: e.scalar_tensor_tensor(out=S_[:, :, 7:8], in0=S_[:, :, 6:7], scalar=8.0, in1=S_[:, :, 3:4], op0=ALU.mult, op1=ALU.add), rg, rg)
            dve(lambda e: e.scalar_tensor_tensor(out=S_[:, :, 7:8], in0=S_[:, :, 7:8], scalar=-1.0, in1=S_[:, :, 2:3], op0=ALU.add, op1=ALU.add), rg, rg)
            dve(lambda e: e.tensor_copy(out=sli[b3_][:, :, 1:2], in_=S_[:, :, 7:8]), rg, [("sli", b3_)])
            dve(lambda e: e.tensor_tensor(out=wsl[b3_].rearrange("p (s o) -> p s o", o=1), in0=S_[:, :, 4:5], in1=S_[:, :, 5:6], op=ALU.add),
                rg, [("wsl", b3_)])

        def gathers(ei):
            b = ei % 2
            b3_ = ei % 4
            for s_ in range(NS):
                def gath(e, b=b, s_=s_, b3_=b3_):
                    if greg[0] is None:
                        greg[0] = e.to_reg(S - 1)
                    return e.indirect_dma_start(out=hg[b][:, s_, :], out_offset=None, in_=h_dram,
                                                in_offset=IndirectOffsetOnAxis(ap=sli[b3_][:, s_, 0:1].bitcast(U32), axis=0),
                                                bounds_check=greg[0], oob_is_err=False)
                P.op("pool", gath, reads=[("slig", b3_)], writes=[("hg", b, s_)], dsem=("ga", b, s_))

        def T_(ei, half):
            b = ei % 2
            for s_ in range(half * 2, half * 2 + 2):
                bank = s_ % 2
                pb = psb(bank)
                pe([tr(pb[:, kc * 128:(kc + 1) * 128], hg[b][:, s_, kc * 128:(kc + 1) * 128], ident_b[:]) for kc in range(KC)],
                   [("hg", b, s_), "ident_b"], [("ps", bank)])
                act(hgT[b][:, :, s_ * 128:(s_ + 1) * 128], pb.rearrange("p (k c) -> p k c", k=KC), AF.Copy, [("ps", bank)], [("hgT", b, s_)])

        def GU_(ei, f):
            b = ei % 2
            wb = ei % NWB
            hgr = [("hgT", b, s_) for s_ in range(NS)]
            gb, ub = (2, 3) if f == 0 else (4, 5)
            pe([mm(ps[gb][:, 0:C], wgb[wb][:, kc, f * 128:(f + 1) * 128], hgT[b][:, kc, :], kc == 0, kc == KC - 1) for kc in range(KC)],
               [("wg", wb)] + hgr, [("ps", gb)])
            pe([mm(ps[ub][:, 0:C], wub[wb][:, kc, f * 128:(f + 1) * 128], hgT[b][:, kc, :], kc == 0, kc == KC - 1) for kc in range(KC)],
               [("wu", wb)] + hgr, [("ps", ub)])
            act(sg2[f], ps[gb][:, 0:C], AF.Silu, [("ps", gb)], [("sg2", f)])
            dve(lambda e, f=f, b=b, ub=ub: e.tensor_tensor(out=hid2[b][:, f, :], in0=sg2[f], in1=ps[ub][:, 0:C], op=ALU.mult),
                [("sg2", f), ("ps", ub)], [("hid2", b, f)])

        def B2(ei):
            b = ei % 2
            wb = ei % NWB
            b3_ = ei % 4
            for s_ in range(NS):
                for dh in range(2):
                    bank = 6 + dh
                    pe([mm(ps[bank][:, :], hid2[b][:, f, s_ * 128:(s_ + 1) * 128], wdb[wb][:, f, dh * 512:(dh + 1) * 512], f == 0, f == 1) for f in range(2)],
                       [("hid2", b, 0), ("hid2", b, 1), ("wd", wb)], [("ps", bank)])
                    act(yout[:, s_, dh * 512:(dh + 1) * 512], ps[bank][:, :], AF.Identity, [("ps", bank), ("wsl", b3_)], [("yout", s_)],
                        scale=wsl[b3_][:, s_:s_ + 1])

                def scat(e, s_=s_, b3_=b3_):
                    if breg[0] is None:
                        breg[0] = e.to_reg(S * 8 - 1)
                    return e.indirect_dma_start(out=ybuf, out_offset=IndirectOffsetOnAxis(ap=sli[b3_][:, s_, 1:2].bitcast(U32), axis=0),
                                                in_=yout[:, s_, :], in_offset=None, bounds_check=breg[0], oob_is_err=False)
                P.op("pool", scat, reads=[("yout", s_), ("sli", b3_)], writes=[], dsem=("sc", s_))

        def valid(e_):
            return 0 <= e_ < NE

        ohs(0, 0)
        for k in range(-3, NE):
            e3, e2, e1, e0 = k + 3, k + 2, k + 1, k
            if valid(e2):
                wb = e2 % NWB
                wload(wgb[wb], wg_d[li, e2].rearrange("(kc p) n -> p kc n", p=128), ("wg", wb), ("e_wg", wb))
                wload(wub[wb], wu_d[li, e2].rearrange("(kc p) n -> p kc n", p=128), ("wu", wb), ("e_wu", wb))
            if valid(e3):
                pe_idx(e3, 0)
                ohs(e3, 1)
            if valid(e2):
                T_(e2, 0)
            if valid(e1):
                GU_(e1, 0)
            if valid(e3):
                pe_idx(e3, 1)
                slot_math(e3)
                gathers(e3)
                if valid(e3 + 1):
                    ohs(e3 + 1, 0)
                slot_math2(e3)
            if valid(e2):
                T_(e2, 1)
            if valid(e1):
                GU_(e1, 1)
            if valid(e0):
                B2(e0)
            if valid(e2):
                wb = e2 % NWB
                wload(wdb[wb], wd_d[li, e2].rearrange("(fc p) n -> p fc n", p=128), ("wd", wb), ("e_wd", wb))
        P.barrier()
        AR.reset()
        ybt = [AR.get([128, 8, D], F32) for _ in range(2)]
        accb = AR.get([128, D], F32)
        yv = ybuf.rearrange("(t p k) d -> t p k d", p=128, k=8)
        for t in range(NT):
            b = t % 2
            dma("sp", ybt[b], yv[t], writes=[("ybt", b)], key=("yl", b))
            dve(lambda e, b=b: e.tensor_tensor(out=accb, in0=ybt[b][:, 0, :], in1=ybt[b][:, 1, :], op=ALU.add), [("ybt", b)], ["accb"])
            for k in range(2, 8):
                dve(lambda e, b=b, k=k: e.tensor_tensor(out=accb, in0=accb, in1=ybt[b][:, k, :], op=ALU.add), [("ybt", b), "accb"], ["accb"])
            dve(lambda e: e.tensor_tensor(out=accb, in0=accb, in1=mod[:, G2, :], op=ALU.mult), ["accb"] + MR(G2), ["accb"])
            dve(lambda e, t=t: e.tensor_tensor(out=x_tok[:, t, :], in0=x_tok[:, t, :], in1=accb, op=ALU.add), ["accb"] + XR(t), XR(t))

    def attn_phase():
        P.barrier()
        AR.reset()
        TG = 256
        kT = AR.get([128, KC, S], BF16)
        v1 = AR.get([128, NT * 16, 66], BF16)
        wA = AR.get([128, 8, D], BF16)
        kmT = AR.get([128, 8, 8], BF16)
        kvmark = AR.off
        wB = AR.get([128, 8, D], BF16)
        hT_G = AR.get([128, KC, TG], BF16)
        t1 = AR.get([128, D], F32)
        hb = AR.get([128, D], BF16)
        kmf = AR.get([128, 8, 8], F32)
        dve(lambda e: e.memset(kmf, 0.0), [], ["kmf0"])
        if not (KV_SKIP & 1):
            dve(lambda e: e.memset(v1[:, :, 64:65], 1.0), [], ["v1ones"])
        wload2(wA, kv_w_k_d.rearrange("(kc p) n -> p kc n", p=128), "wA", "a_w")
        wload2(wB, kv_w_v_d.rearrange("(kc p) n -> p kc n", p=128), "wB", "a_w2")
        for G in range(S // TG):
            for tt in range(2):
                norm_tile(2 * G + tt, 0, 1, hT_G, tt * 128, "hT_G", 7, (t1, hb))
            for fc in range(8 if not (KV_SKIP & 2) else 0):
                pb = fc % 2
                pe([mm(ps[pb][:, 0:TG], wA[:, kc, fc * 128:(fc + 1) * 128], hT_G[:, kc, :], kc == 0, kc == KC - 1) for kc in range(KC)],
                   [("wA", fc // 4), "hT_G"], [("ps", pb)])
                act(kT[:, fc, G * TG:(G + 1) * TG], ps[pb][:, 0:TG], AF.Copy, [("ps", pb), "kmf0"], [("kT", G), ("kmf", G)],
                    accum_out=kmf[:, fc, G:G + 1])
            for tt in range(2 if not (KV_SKIP & 4) else 0):
                t = 2 * G + tt
                for dh in range(2):
                    bank = 2 + (tt * 2 + dh)
                    pe([mm(ps[bank][:, :], hT_G[:, kc, tt * 128:(tt + 1) * 128], wB[:, kc, dh * 512:(dh + 1) * 512], kc == 0, kc == KC - 1) for kc in range(KC)],
                       [("wB", dh), "hT_G"], [("ps", bank)])
                    act(v1[:, t * 16 + dh * 8: t * 16 + (dh + 1) * 8, 0:64], ps[bank][:, :].rearrange("p (h d) -> p h d", h=8), AF.Copy, [("ps", bank)], [("v1", t)])
        if not (KV_SKIP & 8):
            dve(lambda e: e.tensor_scalar(out=kmT, in0=kmf, scalar1=1.0 / 256, scalar2=None, op0=ALU.mult), [("kmf", G) for G in range(8)], ["kmT"])
        return kT, v1, wA, kvmark, kmT

    def attn_phase2(kT, v1, wA, kvmark, kmT):
        P.barrier()
        AR.off = kvmark
        TG = 256
        hT_G = AR.get([128, KC, TG], BF16)
        qT_G = AR.get([128, KC, 2, TG], BF16)
        o_tok = AR.get([128, 2, D], BF16)
        maskT = AR.get([128, TG], BF16)
        sel = [AR.get([128, 8, 128], BF16) for _ in range(2)]
        pT = [AR.get([128, TG], BF16) for _ in range(5)]
        g0 = AR.get([128, 16, 8], F32)
        g1 = AR.get([128, 16, 8], F32)
        eqt = AR.get([128, 16, 8], F32)
        mx = AR.get([128, 16], F32)
        bmb = AR.get([128, 128], BF16)
        rden = AR.get([128, 2], F32)
        t1 = AR.get([128, D], F32)
        hb = AR.get([128, D], BF16)
        tmpf = t1[:, 0:512]
        dve(lambda e: e.memset(qT_G.rearrange("p a b c -> p (a b c)"), 0.0), [], ["qT_G"])
        wqv = b_w_q_d.rearrange("(kc p) n -> p kc n", p=128)
        wov = b_w_o_d.rearrange("(kc p) n -> p kc n", p=128)
        npt = [0]
        bm3 = lambda a: a.rearrange("p (h o) -> p h o", o=1).to_broadcast([128, 16, 8])
        for G in range(S // TG):
            wload2(wA, wqv, "wA", "a_w")
            for tt in range(2):
                norm_tile(2 * G + tt, 0, 1, hT_G, tt * 128, "hT_G", 7, (t1, hb))
            for fc in range(8):
                pb = fc % 2
                pe([mm(ps[pb][:, 0:TG], wA[:, kc, fc * 128:(fc + 1) * 128], hT_G[:, kc, :], kc == 0, kc == KC - 1) for kc in range(KC)],
                   [("wA", fc // 4), "hT_G"], [("ps", pb)])
                act(qT_G[0:64, fc, 0, :], ps[pb][0:64, 0:TG], AF.Copy, [("ps", pb)], ["qT_G"])
                act(qT_G[64:128, fc, 1, :], ps[pb][64:128, 0:TG], AF.Copy, [("ps", pb)], ["qT_G"])
            wload2(wA, wov, "wA", "a_w")
            if ATT_STOP == 1:
                continue
            use_mask = G >= 4
            if use_mask:
                for tt in range(2):
                    if not (GATE_SKIP & 1):
                        pe([mm(ps[2][:, h * 8:(h + 1) * 8], qT_G[:, h // 2, h % 2, tt * 128:(tt + 1) * 128],
                               kmT[:, h // 2, :], True, True) for h in range(16)],
                           ["qT_G", "kmT"], [("ps", 2)])
                    if GATE_SKIP & 2:
                        continue
                    dve(lambda e: e.memset(g0, -1e30), [], ["g0"])
                    dve(lambda e, G=G: e.tensor_copy(out=g0[:, :, 0:G], in_=ps[2][:, 0:128].rearrange("p (h n) -> p h n", n=8)[:, :, 0:G]),
                        [("ps", 2), "g0"], ["g0"])
                    dve(lambda e: e.tensor_reduce(out=mx, in_=g0, axis=AX.X, op=ALU.max), ["g0"], ["mx"])
                    dve(lambda e: e.tensor_tensor(out=eqt, in0=g0, in1=bm3(mx), op=ALU.is_ge), ["g0", "mx"], ["eqt"])
                    dve(lambda e: e.scalar_tensor_tensor(out=g1, in0=eqt, scalar=-1e30, in1=g0, op0=ALU.mult, op1=ALU.add), ["eqt", "g0"], ["g1"])
                    dve(lambda e: e.tensor_reduce(out=mx, in_=g1, axis=AX.X, op=ALU.max), ["g1"], ["mx"])
                    dve(lambda e: e.tensor_tensor(out=eqt, in0=g1, in1=bm3(mx), op=ALU.is_ge), ["g1", "mx"], ["eqt"])
                    dve(lambda e: e.scalar_tensor_tensor(out=g1, in0=eqt, scalar=-1e30, in1=g1, op0=ALU.mult, op1=ALU.add), ["eqt", "g1"], ["g1"])
                    dve(lambda e: e.tensor_reduce(out=mx, in_=g1, axis=AX.X, op=ALU.max), ["g1"], ["mx"])
                    dve(lambda e: e.tensor_tensor(out=eqt, in0=g0, in1=bm3(mx), op=ALU.is_ge), ["g0", "mx"], ["eqt"])
                    dve(lambda e: e.tensor_scalar(out=bmb, in0=eqt.rearrange("p h n -> p (h n)"), scalar1=-1.0, scalar2=-NEG, op0=ALU.add, op1=ALU.mult),
                        ["eqt"], ["bmb"])
                    if GATE_SKIP & 4:
                        continue
                    pe([mm(ps[3][:, 0:128], bmb, ident_b[:], True, True)], ["bmb", "ident_b"], [("ps", 3)])
                    if not (GATE_SKIP & 8):
                        act(maskT[:, tt * 128:(tt + 1) * 128], ps[3][:, 0:128], AF.Copy, [("ps", 3)], ["maskT"])
            njt = 2 * G + 2
            items = [(h, jt) for h in range(16) for jt in range(njt)]

            def mk_sel(h):
                if use_mask:
                    dve(lambda e, h=h: e.tensor_copy(out=sel[h % 2], in_=ident_b[:, h * 8:(h + 1) * 8].rearrange("p (n o) -> p n o", o=1).to_broadcast([128, 8, 128])),
                        ["ident_b"], [("sel", h % 2)])

            def geom(jt, ii):
                qoff = 128 if jt == 2 * G + 1 else 0
                slot = ii % 4
                return qoff, slot, 0

            def S_(h, jt, ii):
                hp, hc = h % 2, h // 2
                qoff, sb, co = geom(jt, ii)
                st = ps[sb][:, co + qoff:co + TG]
                masked = use_mask and jt < 2 * G
                diag = jt >= 2 * G
                fns = [mm(st, kT[:, hc, jt * 128:(jt + 1) * 128], qT_G[:, hc, hp, qoff:TG], True, not (masked or diag))]
                rd = [("kT", jt // 2), "qT_G"]
                if masked:
                    fns.append(mm(st, sel[h % 2][:, jt // 2, :], maskT[:, qoff:TG], False, True))
                    rd += [("sel", h % 2), "maskT"]
                if diag:
                    fns.append(mm(ps[sb][:, co + qoff:co + qoff + 128], ident_b[:], tribias_b[:], False, True))
                    rd += ["ident_b", "tribias_b"]
                pe(fns, rd, [("ps", sb)])

            def EV_(h, jt, ii):
                qoff, sb, co = geom(jt, ii)
                st = ps[sb][:, co + qoff:co + TG]
                obs = [4 + (h % 2) * 2 + qt for qt in range(2)]
                oaccs = [ps[ob_][:, 0:65] for ob_ in obs]
                pi = npt[0] % 5
                npt[0] += 1
                act(pT[pi][:, qoff:TG], st, AF.Exp, [("ps", sb)], [("pT", pi)], scale=0.125)
                fns = []
                for qt in range(qoff // 128, 2):
                    last = (jt == 2 * G + qt)
                    fns.append(mm(oaccs[qt], pT[pi][:, qt * 128:(qt + 1) * 128], v1[:, jt * 16 + h, 0:65], jt == 0, last))
                pe(fns, [("pT", pi), ("v1", jt), "v1ones"], [("ps", obs[qt]) for qt in range(qoff // 128, 2)])
                if jt == njt - 1:
                    for qt in range(2):
                        dve(lambda e, qt=qt, oaccs=oaccs: e.reciprocal(out=rden[:, qt:qt + 1], in_=oaccs[qt][:, 64:65]), [("ps", obs[qt])], [("rden", qt)])
                        dve(lambda e, oaccs=oaccs, qt=qt, h=h: e.tensor_scalar(out=o_tok[:, qt, h * 64:(h + 1) * 64], in0=oaccs[qt][:, 0:64],
                                                                               scalar1=rden[:, qt:qt + 1], scalar2=None, op0=ALU.mult),
                            [("ps", obs[qt]), ("rden", qt)], [("o_tok", qt)])

            LA = 3
            mk_sel(0)
            mk_sel(1)
            for i0 in range(min(LA, len(items))):
                S_(items[i0][0], items[i0][1], i0)
            for ii, (h, jt) in enumerate(items):
                if ii + LA < len(items):
                    hn, jn = items[ii + LA]
                    if jn == 0 and hn + 1 < 16:
                        mk_sel(hn + 1)
                    S_(hn, jn, ii + LA)
                EV_(h, jt, ii)
            for tt in range(2):
                pbk = psb(6 + tt)
                pe([tr(pbk[:, kc * 128:(kc + 1) * 128], o_tok[:, tt, kc * 128:(kc + 1) * 128], ident_b[:]) for kc in range(KC)],
                   [("o_tok", tt), "ident_b"], [("ps", 6 + tt)])
                act(hT_G[:, :, tt * 128:(tt + 1) * 128], pbk.rearrange("p (k c) -> p k c", k=KC), AF.Copy, [("ps", 6 + tt)], ["hT_G"])
            for tt in range(2):
                t = 2 * G + tt
                for dh in range(2):
                    bank = 2 + dh
                    pe([mm(ps[bank][:, :], hT_G[:, kc, tt * 128:(tt + 1) * 128], wA[:, kc, dh * 512:(dh + 1) * 512], kc == 0, kc == KC - 1) for kc in range(KC)],
                       [("wA", dh), "hT_G"], [("ps", bank)])
                    dve(lambda e, bank=bank, dh=dh: e.tensor_tensor(out=tmpf, in0=ps[bank][:, :], in1=mod[:, 2, dh * 512:(dh + 1) * 512], op=ALU.mult),
                        [("ps", bank), ("mod", 2, dh)], ["t1"])
                    dve(lambda e, t=t, dh=dh: e.tensor_tensor(out=x_tok[:, t, dh * 512:(dh + 1) * 512], in0=x_tok[:, t, dh * 512:(dh + 1) * 512], in1=tmpf, op=ALU.add),
                        ["t1", ("x", t, dh)], [("x", t, dh)])

    def final_phase(raw):
        P.barrier()
        AR.reset()
        ob = [AR.get([128, D], F32) for _ in range(2)]
        ov = out_d.rearrange("(t p) d -> p t d", p=128)
        toks = []
        for t in range(NT):
            if raw:
                toks.append(dma("sp", ov[:, t, :], x_tok[:, t, :], reads=XR(t), key="st"))
                continue
            b = t % 2
            ss = stat[:, 0:1]; rs = stat[:, 1:2]
            dve(lambda e: e.memset(ss, 0.0), [], ["ss"])
            act(ob[b], x_tok[:, t, :], AF.Square, XR(t) + ["ss"], [("ob", b), "ss"], accum_out=ss)
            act(rs, ss, AF.Sqrt, ["ss"], ["rs"], bias=1e-6, scale=1.0 / D)
            dve(lambda e: e.reciprocal(out=rs, in_=rs), ["rs"], ["rs"])
            dve(lambda e, t=t, b=b: e.scalar_tensor_tensor(out=ob[b], in0=x_tok[:, t, :], scalar=rs, in1=mod[:, 0, :], op0=ALU.mult, op1=ALU.mult),
                XR(t) + ["rs"] + MR(0), [("ob", b)])
            toks.append(dma("sp", ov[:, t, :], ob[b], reads=[("ob", b)], key=("st", b)))
        P.barrier()

    L0 = [(1, None), (0, 0), (2, None), (4, None), (3, 1), (5, None)]
    L1 = [(1, None), (0, 2), (2, None), (4, None), (3, 3), (5, None)]
    steps = 0
    mods(ada_w_d[0], ada_b_d[0:1, :], 6 * D, L0)
    if stop_after >= 1:
        gmlp_phase()
    if stop_after >= 2:
        (moe_sparse if SPARSE else moe_phase)(0)
    if stop_after >= 3:
        mods(kv_ada_w_d, kv_ada_b_d, 2 * D, [(1, None), (0, 4)])
        if ATT_STOP == -1:
            final_phase(True)
            P.emit()
            return nc, P
        kvstate = attn_phase()
        if ATT_STOP == -2:
            final_phase(True)
            P.emit()
            return nc, P
        keep = kvstate[3]

        mods(ada_w_d[1], ada_b_d[1:2, :], 6 * D, L1, base=keep)
        if ATT_STOP != 0:
            attn_phase2(*kvstate)
    if stop_after >= 4:
        (moe_sparse if SPARSE else moe_phase)(1)
    if stop_after >= 5:
        bcast_row(norms_d[5:6, :], 0)
        final_phase(False)
    else:
        final_phase(True)
    P.emit()
    return nc, P


def host_inputs(inp):
    f = lambda a: np.ascontiguousarray(np.asarray(a, dtype=np.float32))
    sh = {}
    sh["ada_w"] = f(inp["ada_w"])
    sh["ada_b"] = f(inp["ada_b"])
    sh["norms"] = f(np.stack([inp["norm_mix"][0], inp["norm_ffn"][0], inp["norm_mix"][1], inp["norm_ffn"][1],
                              inp["kv_norm"], inp["final_norm"]], axis=0))
    sh["a_w_in"] = f(inp["a_w_in"][0])
    b_in = np.asarray(inp["a_b_in"][0], dtype=np.float32)
    sh["a_b_in_u"] = f(b_in[:2048].reshape(16, 128).T)
    sh["a_b_in_v"] = f(b_in[2048:].reshape(1, 2048))
    sh["a_ln"] = f(np.stack([inp["a_ln_g"][0], inp["a_ln_b"][0]], axis=0))
    sh["a_w_sT"] = f(np.transpose(np.asarray(inp["a_w_s"][0]), (2, 0, 1)))
    sh["a_b_s"] = f(np.asarray(inp["a_b_s"][0]).reshape(1, 1024))
    sh["a_w_out"] = f(inp["a_w_out"][0])
    sh["kv_ada_w"] = f(inp["kv_ada_w"])
    sh["kv_ada_b"] = f(np.asarray(inp["kv_ada_b"]).reshape(1, 2048))
    sh["kv_w_k"] = f(inp["kv_w_k"])
    sh["kv_w_v"] = f(inp["kv_w_v"])
    sh["b_w_q"] = f(inp["b_w_q"][0])
    sh["b_w_o"] = f(inp["b_w_o"][0])
    sh["moe_router"] = f(inp["moe_router"])
    sh["moe_bias"] = f(inp["moe_bias"])
    sh["moe_w_gate"] = f(inp["moe_w_gate"])
    sh["moe_w_up"] = f(inp["moe_w_up"])
    sh["moe_w_down"] = f(inp["moe_w_down"])
    sh["sh_w_gate"] = f(inp["sh_w_gate"])
    sh["sh_w_up"] = f(inp["sh_w_up"])
    sh["sh_w_down"] = f(inp["sh_w_down"])
    sh["ident"] = np.eye(128, dtype=np.float32)
    s_idx = np.arange(128)[:, None]
    t_idx = np.arange(128)[None, :]
    sh["trimask"] = (s_idx <= t_idx).astype(np.float32)
    sh["tribias"] = np.where(t_idx >= s_idx, 0.0, NEG).astype(np.float32)
    sh["iota512"] = np.ascontiguousarray(np.broadcast_to(np.arange(CAP, dtype=np.float32)[None, :], (128, CAP)))
    sh["pidx"] = np.arange(128, dtype=np.float32).reshape(128, 1)
    sh["tix"] = np.ascontiguousarray(np.broadcast_to(np.repeat(np.arange(NT, dtype=np.float32), 64)[None, :], (128, NT * 64)))
    sh["ustrict"] = (np.arange(64)[:, None] < np.arange(64)[None, :]).astype(np.float32)
    sh["lstrict"] = (s_idx < t_idx).astype(np.float32)
    return sh


def kernel(**inp):
    n = 8
    sh = host_inputs(inp)
    x = np.asarray(inp["x"], dtype=np.float32)
    c = np.asarray(inp["c"], dtype=np.float32)
    nc, _ = build()
    in_maps = []
    for b in range(n):
        m = dict(sh)
        m["x"] = np.ascontiguousarray(x[b])
        m["c"] = np.ascontiguousarray(c[b].reshape(8, 128).T)
        in_maps.append(m)
    res = run_bass_kernel_spmd(nc, in_maps, core_ids=list(range(n)))
    return np.stack([np.asarray(r["out"], dtype=np.float32) for r in res.results], axis=0)
```
